# Optimizing a Trainium2 kernel written in Bass

```python
import jax, jax.numpy as jnp
from jax import lax
import numpy as np

D_MODEL = 1024
BATCH = 32
SEQ = 2048
DEPTH = 2

N_HEADS = 8
HEAD_DIM = 64
KV_LATENT = 128
ATTN_SCALE = HEAD_DIM ** -0.5
IDX_HEADS = 8
IDX_DIM = 64
IDX_SCALE = (IDX_HEADS ** -0.5) * (IDX_DIM ** -0.5)
TOPK_MAX = 256
Q_BLOCK = 128
POOL_WINDOWS = (2, 4, 8, 16)
POOL_GROUP = 128
POOL_WIDTH = POOL_GROUP * len(POOL_WINDOWS)
D_FF = 2816
EPS = 1e-6
SPLITS = (N_HEADS * HEAD_DIM, KV_LATENT, IDX_HEADS * IDX_DIM, IDX_DIM, IDX_HEADS, POOL_WIDTH, D_MODEL, D_MODEL)
D_IN = sum(SPLITS)

kernel_name = 'hybrid_dsa_pool_macaron'


def rms_norm(x, g):
    xf = x.astype(jnp.float32)
    y = xf * lax.rsqrt(jnp.mean(xf * xf, axis=-1, keepdims=True) + EPS)
    return (y * g.astype(jnp.float32)).astype(x.dtype)


def swiglu(h, wg, wu, wd):
    return (jax.nn.silu(h @ wg) * (h @ wu)) @ wd


def dsa_attention(q_lat, c_kv, q_idx, k_idx, w_idx):
    B, T = c_kv.shape[0], c_kv.shape[1]
    k_sel = min(TOPK_MAX, T // 4)
    n_blk = T // Q_BLOCK
    key_pos = jnp.arange(T, dtype=jnp.int32)

    def to_blocks(a):
        return jnp.moveaxis(a.reshape((B, n_blk, Q_BLOCK) + a.shape[2:]), 1, 0)

    def one_block(args):
        ql, qi, wi, start = args
        q_pos = start + jnp.arange(Q_BLOCK, dtype=jnp.int32)
        causal = key_pos[None, :] <= q_pos[:, None]
        rel = jax.nn.relu(jnp.einsum('bqjd,bsd->bqjs', qi, k_idx).astype(jnp.float32))
        score = jnp.einsum('bqjs,bqj->bqs', rel, wi.astype(jnp.float32))
        score = jnp.where(causal[None], score, -jnp.inf)
        _, idx = lax.top_k(score, k_sel)
        valid = idx <= q_pos[None, :, None]
        c_sel = jax.vmap(lambda c, i: c[i])(c_kv, idx)
        logits = jnp.einsum('bqhc,bqkc->bqhk', ql, c_sel).astype(jnp.float32) * ATTN_SCALE
        logits = jnp.where(valid[:, :, None, :], logits, -jnp.inf)
        p = jax.nn.softmax(logits, axis=-1).astype(c_sel.dtype)
        return jnp.einsum('bqhk,bqkc->bqhc', p, c_sel)

    starts = jnp.arange(n_blk, dtype=jnp.int32) * Q_BLOCK
    o = lax.map(one_block, (to_blocks(q_lat), to_blocks(q_idx), to_blocks(w_idx), starts))
    return jnp.moveaxis(o, 0, 1).reshape(B, T, N_HEADS, KV_LATENT)


def multiscale_pool(p):
    B, T = p.shape[0], p.shape[1]
    pf = p.astype(jnp.float32).reshape(B, T, len(POOL_WINDOWS), POOL_GROUP)
    cs = jnp.cumsum(pf, axis=1)
    t1 = jnp.arange(1, T + 1, dtype=jnp.float32)
    outs = []
    for g, w in enumerate(POOL_WINDOWS):
        c = cs[:, :, g]
        lag = jnp.pad(c, ((0, 0), (w, 0), (0, 0)))[:, :T]
        cnt = jnp.minimum(t1, float(w))[None, :, None]
        outs.append((c - lag) / cnt - pf[:, :, g])
    return jnp.stack(outs, axis=2).astype(p.dtype)


def setup_inputs(seed: int = 0) -> dict:
    key = jax.random.key(seed)
    ks = jax.random.split(key, 24)
    f32 = jnp.float32

    def nrm(k, shape, fan_in):
        return jax.random.normal(k, shape, f32) * (fan_in ** -0.5)

    def gain(k, shape):
        return 1.0 + 0.05 * jax.random.normal(k, shape, f32)

    G = len(POOL_WINDOWS)
    return {
        'x': jax.random.normal(ks[0], (BATCH, SEQ, D_MODEL), f32),
        'norm_ffn1': gain(ks[1], (DEPTH, D_MODEL)),
        'ffn1_gate': nrm(ks[2], (DEPTH, D_MODEL, D_FF), D_MODEL),
        'ffn1_up': nrm(ks[3], (DEPTH, D_MODEL, D_FF), D_MODEL),
        'ffn1_down': nrm(ks[4], (DEPTH, D_FF, D_MODEL), D_FF),
        'norm_mix': gain(ks[5], (DEPTH, D_MODEL)),
        'w_in': nrm(ks[6], (DEPTH, D_MODEL, D_IN), D_MODEL),
        'norm_kv': gain(ks[7], (DEPTH, KV_LATENT)),
        'w_uk': nrm(ks[8], (DEPTH, KV_LATENT, N_HEADS, HEAD_DIM), KV_LATENT),
        'w_uv': nrm(ks[9], (DEPTH, KV_LATENT, N_HEADS, HEAD_DIM), KV_LATENT),
        'pool_w': nrm(ks[10], (DEPTH, G, POOL_GROUP, POOL_GROUP), POOL_GROUP),
        'pool_scale': gain(ks[11], (DEPTH, POOL_WIDTH)),
        'w_branch_attn': nrm(ks[12], (DEPTH, N_HEADS * HEAD_DIM, D_MODEL), N_HEADS * HEAD_DIM),
        'w_branch_pool': nrm(ks[13], (DEPTH, POOL_WIDTH, D_MODEL), POOL_WIDTH),
        'w_out': nrm(ks[14], (DEPTH, D_MODEL, D_MODEL), D_MODEL),
        'norm_ffn2': gain(ks[15], (DEPTH, D_MODEL)),
        'ffn2_gate': nrm(ks[16], (DEPTH, D_MODEL, D_FF), D_MODEL),
        'ffn2_up': nrm(ks[17], (DEPTH, D_MODEL, D_FF), D_MODEL),
        'ffn2_down': nrm(ks[18], (DEPTH, D_FF, D_MODEL), D_FF),
        'norm_final': gain(ks[19], (D_MODEL,)),
    }


def reference(x, norm_ffn1, ffn1_gate, ffn1_up, ffn1_down, norm_mix, w_in, norm_kv, w_uk, w_uv,
              pool_w, pool_scale, w_branch_attn, w_branch_pool, w_out, norm_ffn2, ffn2_gate,
              ffn2_up, ffn2_down, norm_final):
    B, T = x.shape[0], x.shape[1]
    cuts = np.cumsum(SPLITS)[:-1].tolist()
    for i in range(DEPTH):
        h = rms_norm(x, norm_ffn1[i])
        x = x + 0.5 * swiglu(h, ffn1_gate[i], ffn1_up[i], ffn1_down[i])

        h = rms_norm(x, norm_mix[i])
        z = h @ w_in[i]
        q, c_kv, q_idx, k_idx, w_idx, pool_in, gate_a, gate_b = jnp.split(z, cuts, axis=-1)

        q = q.reshape(B, T, N_HEADS, HEAD_DIM)
        c_kv = rms_norm(c_kv, norm_kv[i])
        q_lat = jnp.einsum('bthd,chd->bthc', q, w_uk[i])
        q_idx = q_idx.reshape(B, T, IDX_HEADS, IDX_DIM)
        o_lat = dsa_attention(q_lat, c_kv, q_idx, k_idx, w_idx * IDX_SCALE)
        attn = jnp.einsum('bthc,chd->bthd', o_lat, w_uv[i]).reshape(B, T, N_HEADS * HEAD_DIM)

        pooled = multiscale_pool(pool_in)
        mixed = jnp.einsum('btgc,gcd->btgd', pooled, pool_w[i]).reshape(B, T, POOL_WIDTH) * pool_scale[i]

        merged = (jax.nn.sigmoid(gate_a) * (attn @ w_branch_attn[i])
                  + jax.nn.sigmoid(gate_b) * (mixed @ w_branch_pool[i]))
        x = x + merged @ w_out[i]

        h = rms_norm(x, norm_ffn2[i])
        x = x + 0.5 * swiglu(h, ffn2_gate[i], ffn2_up[i], ffn2_down[i])
    return rms_norm(x, norm_final)
```

```python
import contextlib
import numpy as np
import concourse.bass as bass
import concourse.mybir as mybir
from concourse.bass_utils import run_bass_kernel_spmd

F32 = mybir.dt.float32
BF16 = mybir.dt.bfloat16
ALU = mybir.AluOpType
ACTF = mybir.ActivationFunctionType
AX = mybir.AxisListType

D = 1024
T = 2048
DEPTH = 2
NH = 8
HD = 64
C = 128
IH = 8
ID = 64
DFF = 2816
NFC = DFF // 128
EPS = 1e-6
ATTN_SCALE = HD ** -0.5
IDX_SCALE = (IH ** -0.5) * (ID ** -0.5)
KSEL = 256
SPLITS = (512, 128, 512, 64, 8, 512, 1024, 1024)
CUTS = np.cumsum((0,) + SPLITS)
O_Q, O_CKV, O_QI, O_KI, O_WI, O_PL, O_GA, O_GB = CUTS[:8]
POOL_W = (2, 4, 8, 16)
NIT = 14
SLOT = 2048
NSLOT = 5
NEG = -1.0e30
MASK_BIG = 30000.0

ENGS = ("pe", "act", "dve", "pool", "sp")
NDMASEM = 8


class Prog:
    def __init__(self, nc):
        self.nc = nc
        self.q = {e: [] for e in ENGS}
        self.nop = {e: 0 for e in ENGS}
        self.waited = {e: {} for e in ENGS}
        self.last_w = {}
        self.readers = {}
        self.dma_cnt = {e: [0] * NDMASEM for e in ENGS}
        self.dma_rr = {e: 0 for e in ENGS}
        self.needed = {e: set() for e in ENGS}

    def _need(self, eng, tok, lst):
        if tok is None:
            return
        sk, val = tok
        if sk == eng and eng == "pe":
            return
        if self.waited[eng].get(sk, 0) >= val:
            return
        self.waited[eng][sk] = val
        lst.append((sk, val))
        if not isinstance(sk, tuple):
            self.needed[sk].add(val)

    @staticmethod
    def _expand(keys):
        out = []
        for k in keys:
            if isinstance(k, str) and len(k) == 4 and k.startswith("pb"):
                out.append(k[:3])
            else:
                out.append(k)
        return out

    def _deps(self, eng, reads, writes, extra):
        reads = self._expand(reads)
        writes = self._expand(writes)
        waits = []
        for k in reads:
            self._need(eng, self.last_w.get(k), waits)
        for k in writes:
            self._need(eng, self.last_w.get(k), waits)
            for t in list(self.readers.get(k, {}).items()):
                self._need(eng, t, waits)
        for t in extra:
            self._need(eng, t, waits)
        return waits

    def _commit(self, tok, reads, writes):
        reads = self._expand(reads)
        writes = self._expand(writes)
        sk, val = tok
        for k in reads:
            d = self.readers.setdefault(k, {})
            if d.get(sk, 0) < val:
                d[sk] = val
        for k in writes:
            self.last_w[k] = tok
            self.readers[k] = {}

    def op(self, eng, fn, reads=(), writes=(), extra=()):
        waits = self._deps(eng, reads, writes, extra)
        self.nop[eng] += 1
        tok = (eng, self.nop[eng])
        self.q[eng].append(("op", fn, waits, self.nop[eng]))
        self._commit(tok, reads, writes)
        return tok

    def dma(self, eng, fn, reads=(), writes=(), extra=()):
        waits = self._deps(eng, reads, writes, extra)
        j = self.dma_rr[eng]
        self.dma_rr[eng] = (j + 1) % NDMASEM
        sk = ("dma", eng, j)
        prev = self.dma_cnt[eng][j]
        if prev > 0:
            self._need(eng, (sk, prev), waits)
        self.dma_cnt[eng][j] = prev + 1
        tok = (sk, prev + 1)
        self.q[eng].append(("dma", fn, waits, sk))
        self._commit(tok, reads, writes)
        return tok

    def wait_all(self, eng, toks):
        waits = []
        for t in toks:
            self._need(eng, t, waits)
        self.q[eng].append(("wait", None, waits, None))

    def emit(self):
        nc = self.nc
        keys = list(ENGS)
        for e in ENGS:
            if any(self.dma_cnt[e]):
                keys += [("dma", e, j) for j in range(NDMASEM)]
        sigval = {}
        for e in ENGS:
            c = 0
            m = {}
            need = self.needed[e]
            for i in range(1, self.nop[e] + 1):
                if i in need:
                    c += 1
                    m[i] = c
            sigval[e] = m
        self.max_sig = {e: len(sigval[e]) for e in ENGS}

        def val_of(sk, v):
            if isinstance(sk, tuple):
                return v * 16
            return sigval[sk][v]

        with contextlib.ExitStack() as st:
            sems = {}
            for i, k in enumerate(keys):
                sems[k] = st.enter_context(nc.semaphore("s%d" % i))

            def mk(engname):
                def body(e):
                    for kind, fn, waits, info in self.q[engname]:
                        for (wsk, v) in waits:
                            e.wait_ge(sems[wsk], val_of(wsk, v))
                        if kind == "op":
                            ins = fn(e)
                            if info in self.needed[engname]:
                                ins.then_inc(sems[engname], 1)
                        elif kind == "dma":
                            fn(e).then_inc(sems[info], 16)
                return body

            with nc.Block() as block:
                block.tensor(mk("pe"))
                block.scalar(mk("act"))
                block.vector(mk("dve"))
                block.gpsimd(mk("pool"))
                block.sync(mk("sp"))


def _inw(w_in, cols):
    sub = w_in[:, cols]
    n = sub.shape[1]
    return sub.reshape(8, 128, n).transpose(1, 0, 2).reshape(128, 8 * n)


def _kmaj(w, nk):
    n = w.shape[1]
    return w.reshape(nk, 128, n).transpose(1, 0, 2).reshape(128, nk * n)


def layer_tiles(inp, l):
    tiles = []
    for f in (1, 2):
        names = {1: ("ffn1_gate", "ffn1_up", "ffn1_down"), 2: ("ffn2_gate", "ffn2_up", "ffn2_down")}[f]
        wg = inp[names[0]][l]
        wu = inp[names[1]][l]
        wd = inp[names[2]][l]
        for fc0 in range(0, NFC, 2):
            for fc in (fc0, fc0 + 1):
                cols = np.arange(fc * 128, (fc + 1) * 128)
                tiles.append(("f%d_gu%d" % (f, fc), np.concatenate([_inw(wg, cols), _inw(wu, cols)], axis=1)))
            tiles.append(("f%d_d%d" % (f, fc0), np.concatenate([wd[fc0 * 128:(fc0 + 1) * 128, :],
                                                              wd[(fc0 + 1) * 128:(fc0 + 2) * 128, :]], axis=1)))
    w_in = inp["w_in"][l]
    ar = np.arange
    kcols = np.concatenate([ar(O_KI, O_KI + 64), ar(O_KI, O_KI + 64)])
    tiles.append(("a0", np.concatenate([_inw(w_in, ar(O_CKV, O_CKV + 128)), _inw(w_in, kcols)], axis=1)))
    tiles.append(("a1", inp["w_uv"][l].reshape(128, 512)))
    for i in range(2):
        tiles.append(("bq%d" % i, np.concatenate([_inw(w_in, ar(O_Q + (2 * i + j) * 128, O_Q + (2 * i + j + 1) * 128))
                                                  for j in range(2)], axis=1)))
    for i in range(2):
        tiles.append(("bqi%d" % i, np.concatenate([_inw(w_in, ar(O_QI + (2 * i + j) * 128, O_QI + (2 * i + j + 1) * 128))
                                                   for j in range(2)], axis=1)))
    wuk = inp["w_uk"][l]
    ukT = wuk.reshape(128, 4, 2, 64).transpose(2, 3, 1, 0).reshape(128, 512)
    tiles.append(("bw", np.concatenate([_inw(w_in, ar(O_WI, O_WI + 8)), ukT], axis=1)))
    for i in range(2):
        tiles.append(("bp%d" % i, np.concatenate([_inw(w_in, ar(O_PL + (2 * i + j) * 128, O_PL + (2 * i + j + 1) * 128))
                                                  for j in range(2)], axis=1)))
    tiles.append(("bpw", inp["pool_w"][l].transpose(1, 0, 2).reshape(128, 512)))
    wba = _kmaj(inp["w_branch_attn"][l], 4).reshape(128, 4, 1024)
    wbp = _kmaj(inp["w_branch_pool"][l], 4).reshape(128, 4, 1024)
    for oc in range(8):
        tiles.append(("m1_%d" % oc, np.concatenate([_inw(w_in, ar(O_GA + oc * 128, O_GA + (oc + 1) * 128)),
                                                   _inw(w_in, ar(O_GB + oc * 128, O_GB + (oc + 1) * 128))], axis=1)))
        tiles.append(("m2_%d" % oc, np.concatenate([wba[:, :, oc * 128:(oc + 1) * 128].reshape(128, 512),
                                                   wbp[:, :, oc * 128:(oc + 1) * 128].reshape(128, 512)], axis=1)))
    wo = _kmaj(inp["w_out"][l], 8).reshape(128, 8, 1024)
    for oc in range(8):
        tiles.append(("o_%d" % oc, wo[:, :, oc * 128:(oc + 1) * 128].reshape(128, 1024)))
    return tiles


def tile_widths():
    w = {}
    for f in (1, 2):
        for fc0 in range(0, NFC, 2):
            w["f%d_gu%d" % (f, fc0)] = 2048
            w["f%d_gu%d" % (f, fc0 + 1)] = 2048
            w["f%d_d%d" % (f, fc0)] = 2048
    w["a0"] = 2048
    w["a1"] = 512
    for i in range(2):
        w["bq%d" % i] = 2048
        w["bqi%d" % i] = 2048
        w["bp%d" % i] = 2048
    w["bw"] = 576
    w["bpw"] = 512
    for oc in range(8):
        w["m1_%d" % oc] = 2048
        w["m2_%d" % oc] = 1024
        w["o_%d" % oc] = 1024
    return w


def tile_order():
    names = []
    for f in (1, 2):
        for fc0 in range(0, NFC, 2):
            names += ["f%d_gu%d" % (f, fc0), "f%d_gu%d" % (f, fc0 + 1), "f%d_d%d" % (f, fc0)]
    names += ["a0", "a1", "bq0", "bq1", "bqi0", "bqi1", "bw", "bp0", "bp1", "bpw"]
    for oc in range(8):
        names += ["m1_%d" % oc, "m2_%d" % oc]
    names += ["o_%d" % oc for oc in range(8)]
    return names


def tile_offsets(depth):
    w = tile_widths()
    off = {}
    c = 0
    for l in range(depth):
        for n in tile_order():
            off[(l, n)] = c
            c += w[n]
    tot = ((c + SLOT - 1) // SLOT) * SLOT
    return off, tot


NCONST = 128 + 128 + 128 + 256 + 128 + (NIT + 1) + 4 * 16


def make_consts():
    c = np.zeros((128, NCONST), np.float32)
    o = 0
    c[:, o:o + 128] = np.eye(128); o += 128
    c[:, o:o + 128] = 1.0; o += 128
    tri = (np.arange(128)[None, :] <= np.arange(128)[:, None]).astype(np.float32)
    c[:, o:o + 128] = tri; o += 128
    c[:, o:o + 128] = 1.0; c[:, o + 128:o + 256] = tri; o += 256
    c[:, o:o + 128] = (tri - 1.0) * 1.0e30; o += 128
    c[:, o:o + NIT + 1] = 2.0 ** (-np.arange(NIT + 1)); o += NIT + 1
    for g, w in enumerate(POOL_W):
        c[:, o:o + 16] = 1.0 / np.minimum(np.arange(16) + 1.0, float(w)); o += 16
    assert o == NCONST
    return c


NVEC_L = 8 + 8 + 8 + 1 + 4


def make_vecs(inp, depth):
    v = np.zeros((128, depth * NVEC_L + 8), np.float32)
    for l in range(depth):
        o = l * NVEC_L
        v[:, o:o + 8] = inp["norm_ffn1"][l].reshape(8, 128).T
        v[:, o + 8:o + 16] = inp["norm_mix"][l].reshape(8, 128).T
        v[:, o + 16:o + 24] = inp["norm_ffn2"][l].reshape(8, 128).T
        v[:, o + 24] = inp["norm_kv"][l]
        v[:, o + 25:o + 29] = inp["pool_scale"][l].reshape(4, 128).T
    v[:, depth * NVEC_L:] = inp["norm_final"].reshape(8, 128).T
    return v


def build(n_seq, depth, stop_after=None, taps=(), mix_stage=99, n_groups=8, extra_dma=0):
    nc = bass.Bass("TRN2", target_bir_lowering=False)
    off, wtot = tile_offsets(depth)
    widths = tile_widths()
    xin = nc.dram_tensor("xT", [n_seq, D, T], F32, kind="ExternalInput").ap()
    wall = nc.dram_tensor("wall", [128, wtot], F32, kind="ExternalInput").ap()
    cst = nc.dram_tensor("consts", [128, NCONST], F32, kind="ExternalInput").ap()
    vec = nc.dram_tensor("vecs", [128, depth * NVEC_L + 8], F32, kind="ExternalInput").ap()
    yout = nc.dram_tensor("yT", [n_seq, D, T], F32, kind="ExternalOutput").ap()
    wscr = nc.dram_tensor("wscr", [128, wtot], BF16, kind="Internal").ap()
    tapout = {}
    for (nm, shape) in taps:
        tapout[nm] = nc.dram_tensor("tap_" + nm, list(shape), F32, kind="ExternalOutput").ap()

    P = Prog(nc)
    with contextlib.ExitStack() as st:
        def sb(name, shape, dt):
            P.sbuf_bytes = getattr(P, "sbuf_bytes", 0) + int(np.prod(shape[1:])) * (2 if dt == BF16 else 4)
            return st.enter_context(nc.sbuf_tensor(name, shape, dt))

        def ps(name, shape, dt):
            return st.enter_context(nc.psum_tensor(name, shape, dt))

        xT = sb("xT_sb", [128, 8, T], F32)
        hT = sb("hT", [128, 8, 1024], BF16)
        act = sb("act", [128, 2, 1024], BF16)
        ring = sb("ring", [128, NSLOT, SLOT], BF16)
        cf = sb("cf", [128, 335], F32)
        vecs = sb("vecs_sb", [128, depth * NVEC_L + 8], F32)
        ident = sb("ident", [128, 128], BF16)
        ones = sb("ones", [128, 128], BF16)
        cm1 = sb("cm1", [128, 256], BF16)
        trib = sb("trib", [128, 128], BF16)
        sq = sb("sq", [128, 2, 512], BF16)
        rt = sb("rt", [128, 512], F32)
        rstd = sb("rstd", [128, 512], F32)
        sg = sb("sg", [128, 2, 512], BF16)
        ckvT = sb("ckvT", [128, T], BF16)
        kidxT = sb("kidxT", [128, T], BF16)
        vaug = sb("vaug", [128, 16, NH * 65], BF16)
        qg = sb("qg", [128, 4, 256], BF16)
        qig = sb("qig", [128, 4, 256], BF16)
        wtok = sb("wtok", [128, 2, 8], F32)
        qlat = sb("qlat", [128, 8, 256], BF16)
        diag = sb("diag", [128, 8, 128], BF16)
        rj = sb("rj", [128, 2, 512], BF16)
        score = sb("score", [128, 2, T], F32)
        mts = sb("mts", [128, 2, T], BF16)
        maskT = sb("maskT", [128, 16, 256], BF16)
        am = sb("am", [128, 1], F32)
        wtab = sb("wtab", [128, NIT + 1], F32)
        probe = sb("probe", [128, 1], F32)
        cnt = sb("cnt", [128, 1], F32)
        aa = sb("aa", [128, 1], F32)
        thr = sb("thr", [128, 1], F32)
        pt = sb("pt", [128, 3, 256], BF16)
        osb = sb("osb", [128, NH * 65], F32)
        rden = sb("rden", [128, 8], F32)
        atok = sb("atok", [128, 512], BF16)
        attnT = sb("attnT", [128, 4, 256], BF16)
        pbuf = sb("pbuf", [128, 4, 272], F32)
        ws1 = sb("ws1", [128, 272], F32)
        ws2 = sb("ws2", [128, 272], F32)
        pooledT = sb("pooledT", [128, 4, 256], BF16)
        mixedT = sb("mixedT", [128, 4, 256], BF16)
        sga = sb("sga", [128, 256], BF16)
        sgb = sb("sgb", [128, 256], BF16)
        m1 = sb("m1", [128, 256], F32)
        m2 = sb("m2", [128, 256], F32)
        mergedT = sb("mergedT", [128, 8, 256], BF16)
        pb = [ps("pb%d" % i, [128, 512], F32) for i in range(7)]
        ptp = ps("ptp", [128, 1024], BF16)

        ring_i = [0]

        def wload(l, name):
            s = ring_i[0] % NSLOT
            ring_i[0] += 1
            w = widths[name]
            o = off[(l, name)]
            key = ("ring", s)
            P.dma("sp", lambda e: e.dma_start(out=ring[:, s, 0:w], in_=wscr[:, o:o + w]),
                  reads=[("wscr", c_) for c_ in range(o // SLOT, (o + w - 1) // SLOT + 1)], writes=[key])
            return s, key

        P.dma("sp", lambda e: e.dma_start(out=score[:, 0, 0:NCONST], in_=cst[:, :]), writes=[("score", 0)])
        P.dma("sp", lambda e: e.dma_start(out=vecs[:], in_=vec[:, :]), writes=["vecs"])
        cst_sb = score[:, 0, :]
        P.op("dve", lambda e: e.tensor_copy(out=ident[:], in_=cst_sb[:, 0:128]), reads=[("score", 0)], writes=["ident"])
        P.op("dve", lambda e: e.tensor_copy(out=ones[:], in_=cst_sb[:, 128:256]), reads=[("score", 0)], writes=["ones"])
        P.op("dve", lambda e: e.tensor_copy(out=trib[:], in_=cst_sb[:, 256:384]), reads=[("score", 0)], writes=["trib"])
        P.op("dve", lambda e: e.tensor_copy(out=cm1[:], in_=cst_sb[:, 384:640]), reads=[("score", 0)], writes=["cm1"])
        P.op("dve", lambda e: e.tensor_copy(out=cf[:, 0:128], in_=cst_sb[:, 256:384]), reads=[("score", 0)], writes=["cf"])
        P.op("dve", lambda e: e.tensor_copy(out=cf[:, 128:335], in_=cst_sb[:, 640:640 + 207]), reads=[("score", 0)], writes=["cf"])
        C_TRI = 0
        C_NEG = 128
        C_P2 = 256
        C_IC = C_P2 + NIT + 1

        nchunk = wtot // SLOT
        cast_engs = ("act", "dve")
        for ci in range(nchunk):
            k = ci % 4
            xkeys = [("x", k, tt) for tt in range(4)]
            s = ci % NSLOT
            P.dma("sp", lambda e, k=k, ci=ci: e.dma_start(out=xT[:, k, :], in_=wall[:, ci * SLOT:(ci + 1) * SLOT]),
                  writes=xkeys)
            ce = cast_engs[ci % 2]
            if ce == "act":
                P.op("act", lambda e, k=k, s=s: e.activation(out=ring[:, s, :], in_=xT[:, k, :], func=ACTF.Copy),
                     reads=xkeys, writes=[("ring", s)])
            else:
                P.op(ce, lambda e, k=k, s=s: e.tensor_copy(out=ring[:, s, :], in_=xT[:, k, :]),
                     reads=xkeys, writes=[("ring", s)])
            P.dma("sp", lambda e, s=s, ci=ci: e.dma_start(out=wscr[:, ci * SLOT:(ci + 1) * SLOT], in_=ring[:, s, :]),
                  reads=[("ring", s)], writes=[("wscr", ci)])

        def rms_to_h(l_vec_off, t0, n, hdst, hkeys, D_=D):
            tt = t0 // 512
            xk = [("x", k, tt) for k in range(8)]
            for k in range(8):
                P.op("act", lambda e, k=k: e.activation(out=sq[:, k % 2, 0:n], in_=xT[:, k, t0:t0 + n], func=ACTF.Square),
                     reads=[xk[k]], writes=[("sq", k % 2)])
                P.op("pe", lambda e, k=k: e.matmul(pb[6][:, 0:n], lhsT=ones[:], rhs=sq[:, k % 2, 0:n],
                                                   start=(k == 0), stop=(k == 7)),
                     reads=["ones", ("sq", k % 2)], writes=["pb6"])
            P.op("act", lambda e: e.activation(out=rt[:, 0:n], in_=pb[6][:, 0:n], func=ACTF.Sqrt,
                                               bias=epsb[:], scale=1.0 / D_),
                 reads=["pb6", "epsb"], writes=["rt"])
            P.op("dve", lambda e: e.reciprocal(out=rstd[:, 0:n], in_=rt[:, 0:n]), reads=["rt"], writes=["rstd"])
            for k in range(8):
                P.op("dve", lambda e, k=k: e.scalar_tensor_tensor(
                    out=hdst(k), in0=xT[:, k, t0:t0 + n], scalar=vecs[:, l_vec_off + k:l_vec_off + k + 1],
                    in1=rstd[:, 0:n], op0=ALU.mult, op1=ALU.mult),
                    reads=[xk[k], "rstd", "vecs"], writes=[hkeys(k)])

        epsb = sb("epsb", [128, 1], F32)
        P.op("pool", lambda e: e.memset(epsb[:], EPS), writes=["epsb"])
        negb = sb("negb", [128, 1], F32)
        P.op("pool", lambda e: e.memset(negb[:], -MASK_BIG), writes=["negb"])

        def tap(nm, src_ap_fn, keys):
            if nm in tapout:
                t = P.dma("sp", lambda e: e.dma_start(out=tapout[nm], in_=src_ap_fn()), reads=keys)
                return t

        def ffn(l, f):
            voff = l * NVEC_L + (0 if f == 1 else 16)
            act1 = mts[:, 0, :].rearrange("p (a b) -> p a b", a=2)
            nunits = NFC // 2
            for half in range(2):
                tb0 = half * 1024
                for j in range(2):
                    rms_to_h(voff, tb0 + j * 512, 512,
                             lambda k, j=j: hT[:, k, j * 512:(j + 1) * 512],
                             lambda k, j=j: ("h", k, j))
                gi = [0]
                di = [0]

                def abuf(u):
                    return act if u % 2 == 0 else act1

                def akey(u, jj, j):
                    return ("act", jj, j) if u % 2 == 0 else ("mts", 0)

                def gu_block(u, jj, j, s, wk):
                    gbk = gi[0] % 2
                    gi[0] += 1
                    gb, ub = pb[gbk], pb[2 + gbk]
                    gk, uk = "pb%d" % gbk, "pb%d" % (2 + gbk)
                    ab = abuf(u)
                    for k in range(8):
                        P.op("pe", lambda e, k=k: e.matmul(
                            gb[:, :], lhsT=ring[:, s, k * 128:(k + 1) * 128], rhs=hT[:, k, j * 512:(j + 1) * 512],
                            start=(k == 0), stop=(k == 7)), reads=[wk, ("h", k, j)], writes=[gk])
                    for k in range(8):
                        P.op("pe", lambda e, k=k: e.matmul(
                            ub[:, :], lhsT=ring[:, s, 1024 + k * 128:1024 + (k + 1) * 128],
                            rhs=hT[:, k, j * 512:(j + 1) * 512], start=(k == 0), stop=(k == 7)),
                            reads=[wk, ("h", k, j)], writes=[uk])
                    P.op("act", lambda e: e.activation(out=sg[:, gbk, :], in_=gb[:, :], func=ACTF.Silu),
                         reads=[gk], writes=[("sg", gbk)])
                    P.op("dve", lambda e: e.tensor_tensor(
                        out=ab[:, jj, j * 512:(j + 1) * 512], in0=sg[:, gbk, :], in1=ub[:, :], op=ALU.mult),
                        reads=[("sg", gbk), uk], writes=[akey(u, jj, j)])

                def d_block(u, j, oc, s, wk):
                    dbk = 4 + di[0] % 2
                    di[0] += 1
                    db, dk = pb[dbk], "pb%d" % dbk
                    ab = abuf(u)
                    tt = (tb0 + j * 512) // 512
                    for jj in range(2):
                        P.op("pe", lambda e, jj=jj: e.matmul(
                            db[:, :], lhsT=ring[:, s, jj * 1024 + oc * 128:jj * 1024 + (oc + 1) * 128],
                            rhs=ab[:, jj, j * 512:(j + 1) * 512], start=(jj == 0), stop=(jj == 1)),
                            reads=[wk, akey(u, jj, j)], writes=[dk])
                    P.op("dve", lambda e: e.scalar_tensor_tensor(
                        out=xT[:, oc, tt * 512:(tt + 1) * 512], in0=db[:, :], scalar=0.5,
                        in1=xT[:, oc, tt * 512:(tt + 1) * 512], op0=ALU.mult, op1=ALU.add),
                        reads=[dk, ("x", oc, tt)], writes=[("x", oc, tt)])

                for u in range(nunits + 1):
                    dlist = [(j, oc) for j in range(2) for oc in range(8)] if u >= 1 else []
                    dstate = [None]

                    def emit_d(n, u=u, dlist=dlist, dstate=dstate):
                        for _ in range(n):
                            if not dlist:
                                return
                            if dstate[0] is None:
                                dstate[0] = wload(l, "f%d_d%d" % (f, 2 * (u - 1)))
                            j, oc = dlist.pop(0)
                            d_block(u - 1, j, oc, dstate[0][0], dstate[0][1])

                    if u < nunits:
                        for jj in range(2):
                            s, wk = wload(l, "f%d_gu%d" % (f, 2 * u + jj))
                            for j in range(2):
                                gu_block(u, jj, j, s, wk)
                                emit_d(4)
                    emit_d(16)

        HB = 512

        def evac(eng, out_ap, in_ap, rkeys, wkeys, scale=None):
            if eng == "act":
                if scale is None:
                    return P.op("act", lambda e: e.activation(out=out_ap, in_=in_ap, func=ACTF.Copy), reads=rkeys, writes=wkeys)
                return P.op("act", lambda e: e.activation(out=out_ap, in_=in_ap, func=ACTF.Copy, scale=scale), reads=rkeys, writes=wkeys)
            if scale is None:
                return P.op(eng, lambda e: e.tensor_copy(out=out_ap, in_=in_ap), reads=rkeys, writes=wkeys)
            return P.op(eng, lambda e: e.tensor_scalar(out=out_ap, in0=in_ap, scalar1=scale, scalar2=None, op0=ALU.mult),
                        reads=rkeys, writes=wkeys)

        def proj256(s, wk, coff, bank, half, hb=HB, n=256):
            bkey = "pb%d%s" % (bank, "LR"[half])
            hk = 1 if hb == HB else 0
            for k in range(8):
                P.op("pe", lambda e, k=k: e.matmul(pb[bank][:, half * 256:half * 256 + n],
                                                   lhsT=ring[:, s, coff + k * 128:coff + (k + 1) * 128],
                                                   rhs=hT[:, k, hb:hb + n], start=(k == 0), stop=(k == 7)),
                     reads=[wk, ("h", k, hk)], writes=[bkey])
            return pb[bank][:, half * 256:half * 256 + n], bkey

        def mixer_A(l):
            voff = l * NVEC_L + 8
            s0, wk0 = wload(l, "a0")
            s1, wk1 = wload(l, "a1")
            for tt in range(4):
                t0 = tt * 512
                rms_to_h(voff, t0, 512, lambda k: hT[:, k, 0:512], lambda k: ("h", k, 0))
                for k in range(8):
                    P.op("pe", lambda e, k=k: e.matmul(pb[0][:, :], lhsT=ring[:, s0, k * 128:(k + 1) * 128], rhs=hT[:, k, 0:512],
                                                       start=(k == 0), stop=(k == 7)), reads=[wk0, ("h", k, 0)], writes=["pb0"])
                for k in range(8):
                    P.op("pe", lambda e, k=k: e.matmul(pb[1][:, :], lhsT=ring[:, s0, 1024 + k * 128:1024 + (k + 1) * 128],
                                                       rhs=hT[:, k, 0:512], start=(k == 0), stop=(k == 7)),
                         reads=[wk0, ("h", k, 0)], writes=["pb1"])
                evac("act", kidxT[:, t0:t0 + 512], pb[1][:, :], ["pb1"], [("kidxT", tt)])
                P.op("act", lambda e: e.activation(out=sq[:, 0, :], in_=pb[0][:, :], func=ACTF.Square),
                     reads=["pb0"], writes=[("sq", 0)])
                evac("dve", score[:, 0, 0:512], pb[0][:, :], ["pb0"], [("score", 0)])
                P.op("pe", lambda e: e.matmul(pb[6][:, :], lhsT=ones[:], rhs=sq[:, 0, :], start=True, stop=True),
                     reads=["ones", ("sq", 0)], writes=["pb6"])
                P.op("act", lambda e: e.activation(out=rt[:, :], in_=pb[6][:, :], func=ACTF.Sqrt, bias=epsb[:], scale=1.0 / C),
                     reads=["pb6", "epsb"], writes=["rt"])
                P.op("dve", lambda e: e.reciprocal(out=rstd[:, :], in_=rt[:, :]), reads=["rt"], writes=["rstd"])
                P.op("dve", lambda e, t0=t0: e.scalar_tensor_tensor(
                    out=ckvT[:, t0:t0 + 512], in0=score[:, 0, 0:512], scalar=vecs[:, l * NVEC_L + 24:l * NVEC_L + 25],
                    in1=rstd[:, :], op0=ALU.mult, op1=ALU.mult), reads=[("score", 0), "rstd", "vecs"], writes=[("ckvT", tt)])
                for b in range(4):
                    sbk = tt * 4 + b
                    vb = 2 + b % 2
                    P.op("pe", lambda e, sbk=sbk, vb=vb: e.matmul(pb[vb][:, :], lhsT=ckvT[:, sbk * 128:(sbk + 1) * 128],
                                                                  rhs=ring[:, s1, 0:512], start=True, stop=True),
                         reads=[("ckvT", tt), wk1], writes=["pb%d" % vb])
                    evac("act", vaug[:, sbk, :].rearrange("p (h e) -> p h e", e=65)[:, :, 0:64],
                         pb[vb][:, :].rearrange("p (h d) -> p h d", d=64), ["pb%d" % vb], [("vaug", tt)])

        def hbase(g):
            return HB + 256 * (g % 2)

        def hkey(g):
            return "h%d" % (1 + g % 2)

        def proj256g(g, s, wk, coff, bank):
            hb = hbase(g)
            hk = hkey(g)
            for k in range(8):
                P.op("pe", lambda e, k=k: e.matmul(pb[bank][:, 0:256], lhsT=ring[:, s, coff + k * 128:coff + (k + 1) * 128],
                                                   rhs=hT[:, k, hb:hb + 256], start=(k == 0), stop=(k == 7)),
                     reads=[wk, (hk, k)], writes=["pb%d" % bank])
            return pb[bank][:, 0:256], "pb%d" % bank

        def mix_prep(l, g):
            voff = l * NVEC_L + 8
            t0 = g * 256
            tbs = (2 * g, 2 * g + 1)
            hb = hbase(g)
            hk = hkey(g)
            rms_to_h(voff, t0, 256, lambda k: hT[:, k, hb:hb + 256], lambda k: (hk, k))
            if tbs[1] < 2:
                return
            for i2 in range(2):
                s, wk = wload(l, "bqi%d" % i2)
                for jj in range(2):
                    i = 2 * i2 + jj
                    src, bkey = proj256g(g, s, wk, jj * 1024, i % 2)
                    evac("act", qig[:, i, :], src, [bkey], [("qig", i)])
            sw, wkw = wload(l, "bw")
            for tbl in range(2):
                for k in range(8):
                    P.op("pe", lambda e, k=k, tbl=tbl: e.matmul(pb[0][:, tbl * 8:tbl * 8 + 8],
                                                               lhsT=hT[:, k, hb + tbl * 128:hb + (tbl + 1) * 128],
                                                               rhs=ring[:, sw, k * 8:(k + 1) * 8], start=(k == 0), stop=(k == 7)),
                         reads=[wkw, (hk, k)], writes=["pb0"])
                evac("dve", wtok[:, tbl, :], pb[0][:, tbl * 8:tbl * 8 + 8], ["pb0"], [("wtok", tbl)], scale=IDX_SCALE)
            for tbl in range(2):
                tb = tbs[tbl]
                n_s = 128 * (tb + 1)
                if tb < 2:
                    continue
                for j in range(8):
                    P.op("dve", lambda e, j=j, tbl=tbl: e.tensor_scalar(out=diag[:, j, :], in0=ident[:], scalar1=wtok[:, tbl, j:j + 1],
                                                                        scalar2=None, op0=ALU.mult),
                         reads=["ident", ("wtok", tbl)], writes=[("diag", j)])
                iits = [(sr0, j) for sr0 in range(0, n_s, 512) for j in range(8)]

                def emit_S(it, tbl=tbl, n_s=n_s, iits=iits):
                    sr0, j = iits[it]
                    ncol = min(512, n_s - sr0)
                    i, r = j // 2, j % 2
                    sbank = 2 + it % 2
                    P.op("pe", lambda e: e.matmul(
                        pb[sbank][:, 0:ncol], lhsT=qig[64 * r:64 * r + 64, i, tbl * 128:(tbl + 1) * 128],
                        rhs=kidxT[64 * r:64 * r + 64, sr0:sr0 + ncol], start=True, stop=True),
                        reads=[("qig", i), ("kidxT", sr0 // 512)], writes=["pb%d" % sbank])

                def emit_acc(it, tbl=tbl, n_s=n_s, iits=iits):
                    sr0, j = iits[it]
                    ncol = min(512, n_s - sr0)
                    sbank = 2 + it % 2
                    ri = it % 2
                    P.op("act", lambda e: e.activation(out=rj[:, ri, 0:ncol], in_=pb[sbank][:, 0:ncol], func=ACTF.Relu),
                         reads=["pb%d" % sbank], writes=[("rj", ri)])
                    P.op("pe", lambda e: e.matmul(pb[4][:, 0:ncol], lhsT=diag[:, j, :], rhs=rj[:, ri, 0:ncol],
                                                  start=(j == 0), stop=(j == 7)),
                         reads=[("diag", j), ("rj", ri)], writes=["pb4"])
                    if j == 7:
                        evac("act", score[:, tbl, sr0:sr0 + ncol], pb[4][:, 0:ncol], ["pb4"], [("score", tbl)])

                emit_S(0)
                for it in range(len(iits)):
                    if it + 1 < len(iits):
                        emit_S(it + 1)
                    emit_acc(it)

        def mix_bis(l, g):
            tbs = (2 * g, 2 * g + 1)
            for tbl in range(2):
                tb = tbs[tbl]
                n_s = 128 * (tb + 1)
                if tb < 2:
                    continue
                sk, mk = ("score", tbl), ("mts", tbl)
                sc = score[:, tbl, :]
                mt = mts[:, tbl, :]
                P.op("dve", lambda e, n_s=n_s, sc=sc, mt=mt: e.tensor_scalar(out=mt[:, 0:n_s], in0=sc[:, 0:n_s], scalar1=1.0, scalar2=None,
                                                                             op0=ALU.mult, op1=ALU.max, accum_out=am[:]),
                     reads=[sk], writes=[mk, "am"])
                P.op("dve", lambda e, n_s=n_s, sc=sc, mt=mt: e.tensor_scalar(out=mt[:, 0:n_s], in0=sc[:, 0:n_s], scalar1=-1.0, scalar2=None,
                                                                             op0=ALU.mult, op1=ALU.max, accum_out=aa[:]),
                     reads=[sk], writes=[mk, "aa"])
                P.op("dve", lambda e: e.tensor_tensor(out=am[:], in0=am[:], in1=aa[:], op=ALU.max), reads=["am", "aa"], writes=["am"])
                d0 = tb * 128
                P.op("dve", lambda e, d0=d0, sc=sc: e.tensor_tensor(out=sc[:, d0:d0 + 128], in0=sc[:, d0:d0 + 128],
                                                                    in1=cf[:, C_TRI:C_TRI + 128], op=ALU.mult), reads=[sk, "cf"], writes=[sk])
                P.op("dve", lambda e, d0=d0, sc=sc: e.tensor_tensor(out=sc[:, d0:d0 + 128], in0=sc[:, d0:d0 + 128],
                                                                    in1=cf[:, C_NEG:C_NEG + 128], op=ALU.add), reads=[sk, "cf"], writes=[sk])
                P.op("dve", lambda e: e.tensor_scalar(out=wtab[:], in0=cf[:, C_P2:C_P2 + NIT + 1], scalar1=am[:, 0:1], scalar2=None,
                                                      op0=ALU.mult), reads=["cf", "am"], writes=["wtab"])
                P.op("dve", lambda e: e.memset(probe[:], 0.0), writes=["probe"])
                for it in range(NIT):
                    P.op("dve", lambda e, n_s=n_s, sc=sc, mt=mt: e.tensor_scalar(out=mt[:, 0:n_s], in0=sc[:, 0:n_s], scalar1=probe[:, 0:1],
                                                                                 scalar2=None, op0=ALU.is_ge, op1=ALU.add, accum_out=cnt[:]),
                         reads=[sk, "probe"], writes=[mk, "cnt"])
                    P.op("dve", lambda e: e.tensor_scalar(out=aa[:], in0=cnt[:], scalar1=KSEL - 0.5, scalar2=0.5,
                                                          op0=ALU.is_ge, op1=ALU.subtract), reads=["cnt"], writes=["aa"])
                    P.op("dve", lambda e, it=it: e.scalar_tensor_tensor(out=probe[:], in0=aa[:], scalar=wtab[:, it:it + 1], in1=probe[:],
                                                                        op0=ALU.mult, op1=ALU.add),
                         reads=["aa", "wtab", "probe"], writes=["probe"])
                P.op("dve", lambda e: e.tensor_tensor(out=thr[:], in0=probe[:], in1=wtab[:, NIT:NIT + 1], op=ALU.subtract),
                     reads=["probe", "wtab"], writes=["thr"])
                P.op("dve", lambda e, n_s=n_s, sc=sc, mt=mt: e.tensor_scalar(out=mt[:, 0:n_s], in0=sc[:, 0:n_s], scalar1=thr[:, 0:1],
                                                                             scalar2=None, op0=ALU.is_ge), reads=[sk, "thr"], writes=[mk])

        def mix_tr(l, g):
            tbs = (2 * g, 2 * g + 1)
            for tbl in range(2):
                tb = tbs[tbl]
                if tb >= 2:
                    msrc, mkey = mts[:, tbl, :], ("mts", tbl)
                elif tb == 1:
                    msrc, mkey = cm1, "cm1"
                else:
                    msrc, mkey = trib, "trib"
                for sb0 in range(0, tb + 1, 4):
                    nb = min(4, tb + 1 - sb0)
                    hf = (sb0 // 4) % 2
                    for b in range(nb):
                        P.op("pe", lambda e, b=b, sb0=sb0, hf=hf, msrc=msrc: e.transpose(
                            out=ptp[:, hf * 512 + b * 128:hf * 512 + (b + 1) * 128], in_=msrc[:, (sb0 + b) * 128:(sb0 + b + 1) * 128],
                            identity=ident[:]), reads=[mkey, "ident"], writes=["ptp"])
                    P.op("act", lambda e, sb0=sb0, nb=nb, hf=hf, tbl=tbl: e.activation(
                        out=maskT[:, sb0:sb0 + nb, tbl * 128:(tbl + 1) * 128],
                        in_=ptp[:, hf * 512:hf * 512 + nb * 128].rearrange("p (b t) -> p b t", t=128),
                        func=ACTF.Identity, scale=MASK_BIG, bias=negb[:]),
                        reads=["ptp", "negb"], writes=[("maskT", tbl)])

        def mix_att(l, g):
            tbs = (2 * g, 2 * g + 1)
            for i2 in range(2):
                s, wk = wload(l, "bq%d" % i2)
                for jj in range(2):
                    i = 2 * i2 + jj
                    src, bkey = proj256g(g, s, wk, jj * 1024, i % 2)
                    evac("act", qg[:, i, :], src, [bkey], [("qg", i)])
            sw, wkw = wload(l, "bw")
            for h in range(8):
                i, r = h // 2, h % 2
                bank = h % 2
                P.op("pe", lambda e, i=i, r=r, bank=bank: e.matmul(pb[bank][:, 0:256],
                                                                   lhsT=ring[64 * r:64 * r + 64, sw, 64 + i * 128:64 + (i + 1) * 128],
                                                                   rhs=qg[64 * r:64 * r + 64, i, :], start=True, stop=True),
                     reads=[wkw, ("qg", i)], writes=["pb%d" % bank])
                evac("act", qlat[:, h, :], pb[bank][:, 0:256], ["pb%d" % bank], [("qlat", h)])
            n_sb = tbs[1] + 1
            its = [(sbk, h) for sbk in range(n_sb) for h in range(8)]

            def emit_L(it):
                sbk, h = its[it]
                c0 = 0 if sbk <= tbs[0] else 128
                lb = it % 2
                ttk = sbk // 4
                P.op("pe", lambda e: e.matmul(pb[lb][:, c0:256], lhsT=ckvT[:, sbk * 128:(sbk + 1) * 128],
                                              rhs=qlat[:, h, c0:256], start=True, stop=False),
                     reads=[("ckvT", ttk), ("qlat", h)], writes=["pb%d" % lb])
                P.op("pe", lambda e: e.matmul(pb[lb][:, c0:256], lhsT=ident[:], rhs=maskT[:, sbk, c0:256],
                                              start=False, stop=True),
                     reads=["ident", ("maskT", 0), ("maskT", 1)], writes=["pb%d" % lb])

            def emit_rest(it):
                sbk, h = its[it]
                c0 = 0 if sbk <= tbs[0] else 128
                lb = it % 2
                pi = it % 3
                ttk = sbk // 4
                P.op("act", lambda e: e.activation(out=pt[:, pi, c0:256], in_=pb[lb][:, c0:256], func=ACTF.Exp, scale=ATTN_SCALE),
                     reads=["pb%d" % lb], writes=[("pt", pi)])
                for tbl in range(2):
                    if sbk > tbs[tbl]:
                        continue
                    ab = 2 + 2 * tbl + h // 4
                    P.op("pe", lambda e, tbl=tbl, ab=ab: e.matmul(
                        pb[ab][:, (h % 4) * 65:(h % 4) * 65 + 65], lhsT=pt[:, pi, tbl * 128:(tbl + 1) * 128],
                        rhs=vaug[:, sbk, h * 65:(h + 1) * 65], start=(sbk == 0 and h % 4 == 0), stop=(sbk == tbs[tbl] and h % 4 == 3)),
                        reads=[("pt", pi), ("vaug", ttk)], writes=["pb%d" % ab])

            emit_L(0)
            for it in range(len(its)):
                if it + 1 < len(its):
                    emit_L(it + 1)
                emit_rest(it)

        def mix_rest(l, g):
            t0 = g * 256
            tt = t0 // 512
            for tbl in range(2):
                for hh in range(2):
                    ab = 2 + 2 * tbl + hh
                    evac("act", osb[:, hh * 260:(hh + 1) * 260], pb[ab][:, 0:260], ["pb%d" % ab], ["osb"])
                P.op("dve", lambda e: e.reciprocal(out=rden[:, :], in_=osb[:, :].rearrange("p (h e) -> p h e", e=65)[:, :, 64]),
                     reads=["osb"], writes=["rden"])
                for h in range(8):
                    P.op("dve", lambda e, h=h: e.tensor_scalar(out=atok[:, h * 64:(h + 1) * 64], in0=osb[:, h * 65:h * 65 + 64],
                                                               scalar1=rden[:, h:h + 1], scalar2=None, op0=ALU.mult),
                         reads=["osb", "rden"], writes=["atok"])
                for i in range(4):
                    P.op("pe", lambda e, i=i: e.transpose(out=ptp[:, i * 128:(i + 1) * 128], in_=atok[:, i * 128:(i + 1) * 128], identity=ident[:]),
                         reads=["atok", "ident"], writes=["ptp"])
                evac("act", attnT[:, :, tbl * 128:(tbl + 1) * 128], ptp[:, 0:512].rearrange("p (b t) -> p b t", t=128),
                     ["ptp"], ["attnT"])
            if g == 0:
                P.op("dve", lambda e: e.memset(pbuf[:, :, 0:16], 0.0), writes=[("pbuf", c_) for c_ in range(4)])
            for i2 in range(2):
                s, wk = wload(l, "bp%d" % i2)
                for jj in range(2):
                    gch = 2 * i2 + jj
                    src, bkey = proj256g(g, s, wk, jj * 1024, gch % 2)
                    evac("act", pbuf[:, gch, 16:272], src, [bkey], [("pbuf", gch)])
            spw, wkpw = wload(l, "bpw")
            for gch in range(4):
                w = POOL_W[gch]
                cur = pbuf[:, gch, :]
                pk = ("pbuf", gch)
                P.op("dve", lambda e, cur=cur: e.tensor_tensor(out=ws1[:, 1:272], in0=cur[:, 1:272], in1=cur[:, 0:271], op=ALU.add),
                     reads=[pk], writes=["ws1"])
                fin, fk = ws1, "ws1"
                if w >= 4:
                    P.op("dve", lambda e: e.tensor_tensor(out=ws2[:, 3:272], in0=ws1[:, 3:272], in1=ws1[:, 1:270], op=ALU.add),
                         reads=["ws1"], writes=["ws2"])
                    fin, fk = ws2, "ws2"
                if w >= 8:
                    P.op("dve", lambda e: e.tensor_tensor(out=ws1[:, 7:272], in0=ws2[:, 7:272], in1=ws2[:, 3:268], op=ALU.add),
                         reads=["ws2"], writes=["ws1"])
                    fin, fk = ws1, "ws1"
                if w >= 16:
                    P.op("dve", lambda e: e.tensor_tensor(out=ws2[:, 15:272], in0=ws1[:, 15:272], in1=ws1[:, 7:264], op=ALU.add),
                         reads=["ws1"], writes=["ws2"])
                    fin, fk = ws2, "ws2"
                P.op("dve", lambda e, fin=fin, cur=cur, gch=gch, w=w: e.scalar_tensor_tensor(
                    out=pooledT[:, gch, :], in0=fin[:, 16:272], scalar=1.0 / w, in1=cur[:, 16:272], op0=ALU.mult, op1=ALU.subtract),
                    reads=[fk, pk], writes=[("pooledT", gch)])
                if g == 0:
                    P.op("dve", lambda e, fin=fin, gch=gch: e.tensor_tensor(out=m1[:, 0:16], in0=fin[:, 16:32],
                                                                          in1=cf[:, C_IC + gch * 16:C_IC + (gch + 1) * 16], op=ALU.mult),
                         reads=[fk, "cf"], writes=["m1"])
                    P.op("dve", lambda e, cur=cur, gch=gch: e.tensor_tensor(out=pooledT[:, gch, 0:16], in0=m1[:, 0:16], in1=cur[:, 16:32],
                                                                          op=ALU.subtract), reads=["m1", pk], writes=[("pooledT", gch)])
                P.op("dve", lambda e, cur=cur: e.tensor_copy(out=cur[:, 0:16], in_=cur[:, 256:272]), reads=[pk], writes=[pk])
                bank = 2 + gch % 2
                P.op("pe", lambda e, gch=gch, bank=bank: e.matmul(pb[bank][:, 0:256], lhsT=ring[:, spw, gch * 128:(gch + 1) * 128],
                                                                  rhs=pooledT[:, gch, :], start=True, stop=True),
                     reads=[wkpw, ("pooledT", gch)], writes=["pb%d" % bank])
                evac("dve", mixedT[:, gch, :], pb[bank][:, 0:256], ["pb%d" % bank], [("mixedT", gch)],
                     scale=vecs[:, l * NVEC_L + 25 + gch:l * NVEC_L + 26 + gch])
            for oc in range(8):
                s1_, wk1_ = wload(l, "m1_%d" % oc)
                s2_, wk2_ = wload(l, "m2_%d" % oc)
                bA, bB, bC = (0, 1, 2) if oc % 2 == 0 else (4, 5, 3)
                ga, gak = proj256g(g, s1_, wk1_, 0, bA)
                gb_, gbk = proj256g(g, s1_, wk1_, 1024, bB)
                for i in range(4):
                    P.op("pe", lambda e, i=i, s2_=s2_, bC=bC: e.matmul(pb[bC][:, 0:256], lhsT=ring[:, s2_, i * 128:(i + 1) * 128],
                                                                    rhs=attnT[:, i, :], start=(i == 0), stop=(i == 3)),
                         reads=[wk2_, "attnT"], writes=["pb%d" % bC])
                for i in range(4):
                    P.op("pe", lambda e, i=i, s2_=s2_, bC=bC: e.matmul(pb[bC][:, 256:512], lhsT=ring[:, s2_, 512 + i * 128:512 + (i + 1) * 128],
                                                                    rhs=mixedT[:, i, :], start=(i == 0), stop=(i == 3)),
                         reads=[wk2_, ("mixedT", i)], writes=["pb%d" % bC])
                P.op("act", lambda e, ga=ga: e.activation(out=sga[:, :], in_=ga, func=ACTF.Sigmoid), reads=[gak], writes=["sga"])
                P.op("act", lambda e, gb_=gb_: e.activation(out=sgb[:, :], in_=gb_, func=ACTF.Sigmoid), reads=[gbk], writes=["sgb"])
                P.op("dve", lambda e, bC=bC: e.tensor_tensor(out=m1[:, :], in0=sga[:, :], in1=pb[bC][:, 0:256], op=ALU.mult),
                     reads=["sga", "pb%d" % bC], writes=["m1"])
                P.op("dve", lambda e, bC=bC: e.tensor_tensor(out=m2[:, :], in0=sgb[:, :], in1=pb[bC][:, 256:512], op=ALU.mult),
                     reads=["sgb", "pb%d" % bC], writes=["m2"])
                P.op("dve", lambda e, oc=oc: e.tensor_tensor(out=mergedT[:, oc, :], in0=m1[:, :], in1=m2[:, :], op=ALU.add),
                     reads=["m1", "m2"], writes=[("mergedT", oc)])
            for oc in range(8):
                s, wk = wload(l, "o_%d" % oc)
                bank = 4 + oc % 2
                for k in range(8):
                    P.op("pe", lambda e, k=k, bank=bank, s=s: e.matmul(pb[bank][:, 0:256], lhsT=ring[:, s, k * 128:(k + 1) * 128],
                                                                  rhs=mergedT[:, k, :], start=(k == 0), stop=(k == 7)),
                         reads=[wk, ("mergedT", k)], writes=["pb%d" % bank])
                P.op("dve", lambda e, oc=oc, bank=bank: e.tensor_tensor(out=xT[:, oc, t0:t0 + 256], in0=pb[bank][:, 0:256],
                                                                        in1=xT[:, oc, t0:t0 + 256], op=ALU.add),
                     reads=["pb%d" % bank, ("x", oc, tt)], writes=[("x", oc, tt)])

        def mixer(l):
            mixer_A(l)
            mix_prep(l, 0)
            mix_bis(l, 0)
            mix_tr(l, 0)
            for g in range(n_groups):
                if g + 1 < n_groups:
                    mix_prep(l, g + 1)
                    mix_bis(l, g + 1)
                mix_att(l, g)
                if g + 1 < n_groups:
                    mix_tr(l, g + 1)
                mix_rest(l, g)

        P.op("pool", lambda e: e.memset(vaug[:, :, :], 1.0), writes=[("vaug", c_) for c_ in range(4)])

        final_toks = []
        for sq_i in range(n_seq):
            for k in range(8):
                P.dma("sp", lambda e, k=k, sq_i=sq_i: e.dma_start(out=xT[:, k, :], in_=xin[sq_i, k * 128:(k + 1) * 128, :]),
                      writes=[("x", k, tt) for tt in range(4)])
            for l in range(depth):
                ffn(l, 1)
                if stop_after == "ffn1":
                    break
                mixer(l)
                if stop_after == "mix":
                    break
                ffn(l, 2)
            voff = depth * NVEC_L
            for tt in range(4):
                rms_to_h_dummy = None
                xk = [("x", k, tt) for k in range(8)]
                t0 = tt * 512
                for k in range(8):
                    P.op("act", lambda e, k=k, t0=t0: e.activation(out=sq[:, k % 2, :], in_=xT[:, k, t0:t0 + 512], func=ACTF.Square),
                         reads=[xk[k]], writes=[("sq", k % 2)])
                    P.op("pe", lambda e, k=k: e.matmul(pb[6][:, :], lhsT=ones[:], rhs=sq[:, k % 2, :],
                                                       start=(k == 0), stop=(k == 7)),
                         reads=["ones", ("sq", k % 2)], writes=["pb6"])
                P.op("act", lambda e: e.activation(out=rt[:, :], in_=pb[6][:, :], func=ACTF.Sqrt, bias=epsb[:], scale=1.0 / D),
                     reads=["pb6", "epsb"], writes=["rt"])
                P.op("dve", lambda e: e.reciprocal(out=rstd[:, :], in_=rt[:, :]), reads=["rt"], writes=["rstd"])
                for k in range(8):
                    oi = k % 2
                    if stop_after is None:
                        P.op("dve", lambda e, k=k, t0=t0, oi=oi: e.scalar_tensor_tensor(
                            out=score[:, 1, oi * 512:(oi + 1) * 512], in0=xT[:, k, t0:t0 + 512], scalar=vecs[:, voff + k:voff + k + 1],
                            in1=rstd[:, :], op0=ALU.mult, op1=ALU.mult),
                            reads=[xk[k], "rstd", "vecs"], writes=[("ostg", oi), ("score", 1)])
                    else:
                        P.op("dve", lambda e, k=k, t0=t0, oi=oi: e.tensor_copy(out=score[:, 1, oi * 512:(oi + 1) * 512], in_=xT[:, k, t0:t0 + 512]),
                             reads=[xk[k]], writes=[("ostg", oi)])
                    final_toks.append(P.dma("sp", lambda e, k=k, t0=t0, oi=oi, sq_i=sq_i: e.dma_start(
                        out=yout[sq_i, k * 128:(k + 1) * 128, t0:t0 + 512], in_=score[:, 1, oi * 512:(oi + 1) * 512]),
                        reads=[("ostg", oi)]))
        P.wait_all("sp", final_toks)
        P.emit()
    return nc, P


N_CORES = 8


def pack_weights(inp, depth):
    off, wtot = tile_offsets(depth)
    wall = np.zeros((128, wtot), np.float32)
    for l in range(depth):
        for name, arr in layer_tiles(inp, l):
            o = off[(l, name)]
            assert arr.shape == (128, tile_widths()[name]), (name, arr.shape)
            wall[:, o:o + arr.shape[1]] = arr
    return wall


def kernel(**inputs):
    inp = {k: np.asarray(v) for k, v in inputs.items()}
    x = inp["x"]
    B = x.shape[0]
    n_seq = B // N_CORES
    nc, _ = build(n_seq, DEPTH)
    wall = pack_weights(inp, DEPTH)
    consts = make_consts()
    vecs = make_vecs(inp, DEPTH)
    xT = np.ascontiguousarray(x.transpose(0, 2, 1))
    in_maps = []
    for c in range(N_CORES):
        in_maps.append({"xT": xT[c * n_seq:(c + 1) * n_seq], "wall": wall, "consts": consts, "vecs": vecs})
    res = run_bass_kernel_spmd(nc, in_maps, core_ids=list(range(N_CORES)))
    yT = np.concatenate([r["yT"] for r in res.results], axis=0)
    return np.ascontiguousarray(yT.transpose(0, 2, 1))
```

```python
import contextlib
import numpy as np
import concourse.bass as bass
import concourse.mybir as mybir
from concourse.bass_utils import run_bass_kernel_spmd

F32 = mybir.dt.float32
BF16 = mybir.dt.bfloat16
ALU = mybir.AluOpType
ACTF = mybir.ActivationFunctionType
AX = mybir.AxisListType

D = 1024
T = 2048
DEPTH = 2
NH = 8
HD = 64
C = 128
IH = 8
ID = 64
DFF = 2816
NFC = DFF // 128
EPS = 1e-6
ATTN_SCALE = HD ** -0.5
IDX_SCALE = (IH ** -0.5) * (ID ** -0.5)
KSEL = 256
SPLITS = (512, 128, 512, 64, 8, 512, 1024, 1024)
CUTS = np.cumsum((0,) + SPLITS)
O_Q, O_CKV, O_QI, O_KI, O_WI, O_PL, O_GA, O_GB = CUTS[:8]
POOL_W = (2, 4, 8, 16)
NIT = 14
SLOT = 2048
NSLOT = 5
NEG = -1.0e30
MASK_BIG = 30000.0

ENGS = ("pe", "act", "dve", "pool", "sp")
NDMASEM = 8


class Prog:
    def __init__(self, nc):
        self.nc = nc
        self.q = {e: [] for e in ENGS}
        self.nop = {e: 0 for e in ENGS}
        self.waited = {e: {} for e in ENGS}
        self.last_w = {}
        self.readers = {}
        self.dma_cnt = {e: [0] * NDMASEM for e in ENGS}
        self.dma_rr = {e: 0 for e in ENGS}
        self.needed = {e: set() for e in ENGS}

    def _need(self, eng, tok, lst):
        if tok is None:
            return
        sk, val = tok
        if sk == eng and eng == "pe":
            return
        if self.waited[eng].get(sk, 0) >= val:
            return
        self.waited[eng][sk] = val
        lst.append((sk, val))
        if not isinstance(sk, tuple):
            self.needed[sk].add(val)

    @staticmethod
    def _expand(keys):
        out = []
        for k in keys:
            if isinstance(k, str) and len(k) == 4 and k.startswith("pb"):
                out.append(k[:3])
            else:
                out.append(k)
        return out

    def _deps(self, eng, reads, writes, extra):
        reads = self._expand(reads)
        writes = self._expand(writes)
        waits = []
        for k in reads:
            self._need(eng, self.last_w.get(k), waits)
        for k in writes:
            self._need(eng, self.last_w.get(k), waits)
            for t in list(self.readers.get(k, {}).items()):
                self._need(eng, t, waits)
        for t in extra:
            self._need(eng, t, waits)
        return waits

    def _commit(self, tok, reads, writes):
        reads = self._expand(reads)
        writes = self._expand(writes)
        sk, val = tok
        for k in reads:
            d = self.readers.setdefault(k, {})
            if d.get(sk, 0) < val:
                d[sk] = val
        for k in writes:
            self.last_w[k] = tok
            self.readers[k] = {}

    def op(self, eng, fn, reads=(), writes=(), extra=()):
        waits = self._deps(eng, reads, writes, extra)
        self.nop[eng] += 1
        tok = (eng, self.nop[eng])
        self.q[eng].append(("op", fn, waits, self.nop[eng]))
        self._commit(tok, reads, writes)
        return tok

    def dma(self, eng, fn, reads=(), writes=(), extra=()):
        waits = self._deps(eng, reads, writes, extra)
        j = self.dma_rr[eng]
        self.dma_rr[eng] = (j + 1) % NDMASEM
        sk = ("dma", eng, j)
        prev = self.dma_cnt[eng][j]
        if prev > 0:
            self._need(eng, (sk, prev), waits)
        self.dma_cnt[eng][j] = prev + 1
        tok = (sk, prev + 1)
        self.q[eng].append(("dma", fn, waits, sk))
        self._commit(tok, reads, writes)
        return tok

    def wait_all(self, eng, toks):
        waits = []
        for t in toks:
            self._need(eng, t, waits)
        self.q[eng].append(("wait", None, waits, None))

    def emit(self):
        nc = self.nc
        keys = list(ENGS)
        for e in ENGS:
            if any(self.dma_cnt[e]):
                keys += [("dma", e, j) for j in range(NDMASEM)]
        sigval = {}
        for e in ENGS:
            c = 0
            m = {}
            need = self.needed[e]
            for i in range(1, self.nop[e] + 1):
                if i in need:
                    c += 1
                    m[i] = c
            sigval[e] = m
        self.max_sig = {e: len(sigval[e]) for e in ENGS}

        def val_of(sk, v):
            if isinstance(sk, tuple):
                return v * 16
            return sigval[sk][v]

        with contextlib.ExitStack() as st:
            sems = {}
            for i, k in enumerate(keys):
                sems[k] = st.enter_context(nc.semaphore("s%d" % i))

            def mk(engname):
                def body(e):
                    for kind, fn, waits, info in self.q[engname]:
                        for (wsk, v) in waits:
                            e.wait_ge(sems[wsk], val_of(wsk, v))
                        if kind == "op":
                            ins = fn(e)
                            if info in self.needed[engname]:
                                ins.then_inc(sems[engname], 1)
                        elif kind == "dma":
                            fn(e).then_inc(sems[info], 16)
                return body

            with nc.Block() as block:
                block.tensor(mk("pe"))
                block.scalar(mk("act"))
                block.vector(mk("dve"))
                block.gpsimd(mk("pool"))
                block.sync(mk("sp"))


def _inw(w_in, cols):
    sub = w_in[:, cols]
    n = sub.shape[1]
    return sub.reshape(8, 128, n).transpose(1, 0, 2).reshape(128, 8 * n)


def _kmaj(w, nk):
    n = w.shape[1]
    return w.reshape(nk, 128, n).transpose(1, 0, 2).reshape(128, nk * n)


def layer_tiles(inp, l):
    tiles = []
    for f in (1, 2):
        names = {1: ("ffn1_gate", "ffn1_up", "ffn1_down"), 2: ("ffn2_gate", "ffn2_up", "ffn2_down")}[f]
        wg = inp[names[0]][l]
        wu = inp[names[1]][l]
        wd = inp[names[2]][l]
        for fc0 in range(0, NFC, 2):
            for fc in (fc0, fc0 + 1):
                cols = np.arange(fc * 128, (fc + 1) * 128)
                tiles.append(("f%d_gu%d" % (f, fc), np.concatenate([_inw(wg, cols), _inw(wu, cols)], axis=1)))
            tiles.append(("f%d_d%d" % (f, fc0), np.concatenate([wd[fc0 * 128:(fc0 + 1) * 128, :],
                                                              wd[(fc0 + 1) * 128:(fc0 + 2) * 128, :]], axis=1)))
    w_in = inp["w_in"][l]
    ar = np.arange
    kcols = np.concatenate([ar(O_KI, O_KI + 64), ar(O_KI, O_KI + 64)])
    tiles.append(("a0", np.concatenate([_inw(w_in, ar(O_CKV, O_CKV + 128)), _inw(w_in, kcols)], axis=1)))
    tiles.append(("a1", inp["w_uv"][l].reshape(128, 512)))
    for i in range(2):
        tiles.append(("bq%d" % i, np.concatenate([_inw(w_in, ar(O_Q + (2 * i + j) * 128, O_Q + (2 * i + j + 1) * 128))
                                                  for j in range(2)], axis=1)))
    for i in range(2):
        tiles.append(("bqi%d" % i, np.concatenate([_inw(w_in, ar(O_QI + (2 * i + j) * 128, O_QI + (2 * i + j + 1) * 128))
                                                   for j in range(2)], axis=1)))
    wuk = inp["w_uk"][l]
    ukT = wuk.reshape(128, 4, 2, 64).transpose(2, 3, 1, 0).reshape(128, 512)
    tiles.append(("bw", np.concatenate([_inw(w_in, ar(O_WI, O_WI + 8)), ukT], axis=1)))
    for i in range(2):
        tiles.append(("bp%d" % i, np.concatenate([_inw(w_in, ar(O_PL + (2 * i + j) * 128, O_PL + (2 * i + j + 1) * 128))
                                                  for j in range(2)], axis=1)))
    tiles.append(("bpw", inp["pool_w"][l].transpose(1, 0, 2).reshape(128, 512)))
    wba = _kmaj(inp["w_branch_attn"][l], 4).reshape(128, 4, 1024)
    wbp = _kmaj(inp["w_branch_pool"][l], 4).reshape(128, 4, 1024)
    for oc in range(8):
        tiles.append(("m1_%d" % oc, np.concatenate([_inw(w_in, ar(O_GA + oc * 128, O_GA + (oc + 1) * 128)),
                                                   _inw(w_in, ar(O_GB + oc * 128, O_GB + (oc + 1) * 128))], axis=1)))
        tiles.append(("m2_%d" % oc, np.concatenate([wba[:, :, oc * 128:(oc + 1) * 128].reshape(128, 512),
                                                   wbp[:, :, oc * 128:(oc + 1) * 128].reshape(128, 512)], axis=1)))
    wo = _kmaj(inp["w_out"][l], 8).reshape(128, 8, 1024)
    for oc in range(8):
        tiles.append(("o_%d" % oc, wo[:, :, oc * 128:(oc + 1) * 128].reshape(128, 1024)))
    return tiles


def tile_widths():
    w = {}
    for f in (1, 2):
        for fc0 in range(0, NFC, 2):
            w["f%d_gu%d" % (f, fc0)] = 2048
            w["f%d_gu%d" % (f, fc0 + 1)] = 2048
            w["f%d_d%d" % (f, fc0)] = 2048
    w["a0"] = 2048
    w["a1"] = 512
    for i in range(2):
        w["bq%d" % i] = 2048
        w["bqi%d" % i] = 2048
        w["bp%d" % i] = 2048
    w["bw"] = 576
    w["bpw"] = 512
    for oc in range(8):
        w["m1_%d" % oc] = 2048
        w["m2_%d" % oc] = 1024
        w["o_%d" % oc] = 1024
    return w


def tile_order():
    names = []
    for f in (1, 2):
        for fc0 in range(0, NFC, 2):
            names += ["f%d_gu%d" % (f, fc0), "f%d_gu%d" % (f, fc0 + 1), "f%d_d%d" % (f, fc0)]
    names += ["a0", "a1", "bq0", "bq1", "bqi0", "bqi1", "bw", "bp0", "bp1", "bpw"]
    for oc in range(8):
        names += ["m1_%d" % oc, "m2_%d" % oc]
    names += ["o_%d" % oc for oc in range(8)]
    return names


def tile_offsets(depth):
    w = tile_widths()
    off = {}
    c = 0
    for l in range(depth):
        for n in tile_order():
            off[(l, n)] = c
            c += w[n]
    tot = ((c + SLOT - 1) // SLOT) * SLOT
    return off, tot


NCONST = 128 + 128 + 128 + 256 + 128 + (NIT + 1) + 4 * 16


def make_consts():
    c = np.zeros((128, NCONST), np.float32)
    o = 0
    c[:, o:o + 128] = np.eye(128); o += 128
    c[:, o:o + 128] = 1.0; o += 128
    tri = (np.arange(128)[None, :] <= np.arange(128)[:, None]).astype(np.float32)
    c[:, o:o + 128] = tri; o += 128
    c[:, o:o + 128] = 1.0; c[:, o + 128:o + 256] = tri; o += 256
    c[:, o:o + 128] = (tri - 1.0) * 1.0e30; o += 128
    c[:, o:o + NIT + 1] = 2.0 ** (-np.arange(NIT + 1)); o += NIT + 1
    for g, w in enumerate(POOL_W):
        c[:, o:o + 16] = 1.0 / np.minimum(np.arange(16) + 1.0, float(w)); o += 16
    assert o == NCONST
    return c


NVEC_L = 8 + 8 + 8 + 1 + 4


def make_vecs(inp, depth):
    v = np.zeros((128, depth * NVEC_L + 8), np.float32)
    for l in range(depth):
        o = l * NVEC_L
        v[:, o:o + 8] = inp["norm_ffn1"][l].reshape(8, 128).T
        v[:, o + 8:o + 16] = inp["norm_mix"][l].reshape(8, 128).T
        v[:, o + 16:o + 24] = inp["norm_ffn2"][l].reshape(8, 128).T
        v[:, o + 24] = inp["norm_kv"][l]
        v[:, o + 25:o + 29] = inp["pool_scale"][l].reshape(4, 128).T
    v[:, depth * NVEC_L:] = inp["norm_final"].reshape(8, 128).T
    return v


def build(n_seq, depth, stop_after=None, taps=(), mix_stage=99, n_groups=8, extra_dma=0):
    nc = bass.Bass("TRN2", target_bir_lowering=False)
    off, wtot = tile_offsets(depth)
    widths = tile_widths()
    xin = nc.dram_tensor("xT", [n_seq, D, T], F32, kind="ExternalInput").ap()
    wall = nc.dram_tensor("wall", [128, wtot], F32, kind="ExternalInput").ap()
    cst = nc.dram_tensor("consts", [128, NCONST], F32, kind="ExternalInput").ap()
    vec = nc.dram_tensor("vecs", [128, depth * NVEC_L + 8], F32, kind="ExternalInput").ap()
    yout = nc.dram_tensor("yT", [n_seq, D, T], F32, kind="ExternalOutput").ap()
    wscr = nc.dram_tensor("wscr", [128, wtot], BF16, kind="Internal").ap()
    tapout = {}
    for (nm, shape) in taps:
        tapout[nm] = nc.dram_tensor("tap_" + nm, list(shape), F32, kind="ExternalOutput").ap()

    P = Prog(nc)
    with contextlib.ExitStack() as st:
        def sb(name, shape, dt):
            P.sbuf_bytes = getattr(P, "sbuf_bytes", 0) + int(np.prod(shape[1:])) * (2 if dt == BF16 else 4)
            return st.enter_context(nc.sbuf_tensor(name, shape, dt))

        def ps(name, shape, dt):
            return st.enter_context(nc.psum_tensor(name, shape, dt))

        xT = sb("xT_sb", [128, 8, T], F32)
        hT = sb("hT", [128, 8, 1024], BF16)
        act = sb("act", [128, 2, 1024], BF16)
        ring = sb("ring", [128, NSLOT, SLOT], BF16)
        cf = sb("cf", [128, 335], F32)
        vecs = sb("vecs_sb", [128, depth * NVEC_L + 8], F32)
        ident = sb("ident", [128, 128], BF16)
        ones = sb("ones", [128, 128], BF16)
        cm1 = sb("cm1", [128, 256], BF16)
        trib = sb("trib", [128, 128], BF16)
        sq = sb("sq", [128, 2, 512], BF16)
        rt = sb("rt", [128, 512], F32)
        rstd = sb("rstd", [128, 512], F32)
        sg = sb("sg", [128, 2, 512], BF16)
        ckvT = sb("ckvT", [128, T], BF16)
        kidxT = sb("kidxT", [128, T], BF16)
        vaug = sb("vaug", [128, 16, NH * 65], BF16)
        qg = sb("qg", [128, 4, 256], BF16)
        qig = sb("qig", [128, 4, 256], BF16)
        wtok = sb("wtok", [128, 2, 8], F32)
        qlat = sb("qlat", [128, 8, 256], BF16)
        diag = sb("diag", [128, 8, 128], BF16)
        rj = sb("rj", [128, 2, 512], BF16)
        score = sb("score", [128, 2, T], F32)
        mts = sb("mts", [128, 2, T], BF16)
        maskT = sb("maskT", [128, 16, 256], BF16)
        am = sb("am", [128, 1], F32)
        wtab = sb("wtab", [128, NIT + 1], F32)
        probe = sb("probe", [128, 1], F32)
        cnt = sb("cnt", [128, 1], F32)
        aa = sb("aa", [128, 1], F32)
        thr = sb("thr", [128, 1], F32)
        pt = sb("pt", [128, 3, 256], BF16)
        osb = sb("osb", [128, NH * 65], F32)
        rden = sb("rden", [128, 8], F32)
        atok = sb("atok", [128, 512], BF16)
        attnT = sb("attnT", [128, 4, 256], BF16)
        pbuf = sb("pbuf", [128, 4, 272], F32)
        ws1 = sb("ws1", [128, 272], F32)
        ws2 = sb("ws2", [128, 272], F32)
        pooledT = sb("pooledT", [128, 4, 256], BF16)
        mixedT = sb("mixedT", [128, 4, 256], BF16)
        sga = sb("sga", [128, 256], BF16)
        sgb = sb("sgb", [128, 256], BF16)
        m1 = sb("m1", [128, 256], F32)
        m2 = sb("m2", [128, 256], F32)
        mergedT = sb("mergedT", [128, 8, 256], BF16)
        pb = [ps("pb%d" % i, [128, 512], F32) for i in range(7)]
        ptp = ps("ptp", [128, 1024], BF16)

        ring_i = [0]

        def wload(l, name):
            s = ring_i[0] % NSLOT
            ring_i[0] += 1
            w = widths[name]
            o = off[(l, name)]
            key = ("ring", s)
            P.dma("sp", lambda e: e.dma_start(out=ring[:, s, 0:w], in_=wscr[:, o:o + w]),
                  reads=[("wscr", c_) for c_ in range(o // SLOT, (o + w - 1) // SLOT + 1)], writes=[key])
            return s, key

        P.dma("sp", lambda e: e.dma_start(out=score[:, 0, 0:NCONST], in_=cst[:, :]), writes=[("score", 0)])
        P.dma("sp", lambda e: e.dma_start(out=vecs[:], in_=vec[:, :]), writes=["vecs"])
        cst_sb = score[:, 0, :]
        P.op("dve", lambda e: e.tensor_copy(out=ident[:], in_=cst_sb[:, 0:128]), reads=[("score", 0)], writes=["ident"])
        P.op("dve", lambda e: e.tensor_copy(out=ones[:], in_=cst_sb[:, 128:256]), reads=[("score", 0)], writes=["ones"])
        P.op("dve", lambda e: e.tensor_copy(out=trib[:], in_=cst_sb[:, 256:384]), reads=[("score", 0)], writes=["trib"])
        P.op("dve", lambda e: e.tensor_copy(out=cm1[:], in_=cst_sb[:, 384:640]), reads=[("score", 0)], writes=["cm1"])
        P.op("dve", lambda e: e.tensor_copy(out=cf[:, 0:128], in_=cst_sb[:, 256:384]), reads=[("score", 0)], writes=["cf"])
        P.op("dve", lambda e: e.tensor_copy(out=cf[:, 128:335], in_=cst_sb[:, 640:640 + 207]), reads=[("score", 0)], writes=["cf"])
        C_TRI = 0
        C_NEG = 128
        C_P2 = 256
        C_IC = C_P2 + NIT + 1

        nchunk = wtot // SLOT
        cast_engs = ("act", "dve")
        def p_load(ci):
            k = ci % 4
            P.dma("sp", lambda e: e.dma_start(out=xT[:, k, :], in_=wall[:, ci * SLOT:(ci + 1) * SLOT]),
                  writes=[("x", k, tt) for tt in range(4)])

        def p_cast_store(ci):
            k = ci % 4
            xkeys = [("x", k, tt) for tt in range(4)]
            s = ci % NSLOT
            ce = cast_engs[ci % 2]
            if ce == "act":
                P.op("act", lambda e: e.activation(out=ring[:, s, :], in_=xT[:, k, :], func=ACTF.Copy),
                     reads=xkeys, writes=[("ring", s)])
            else:
                P.op(ce, lambda e: e.tensor_copy(out=ring[:, s, :], in_=xT[:, k, :]),
                     reads=xkeys, writes=[("ring", s)])
            P.dma("sp", lambda e: e.dma_start(out=wscr[:, ci * SLOT:(ci + 1) * SLOT], in_=ring[:, s, :]),
                  reads=[("ring", s)], writes=[("wscr", ci)])

        for ci in range(min(4, nchunk)):
            p_load(ci)
        for ci in range(nchunk):
            p_cast_store(ci)
            if ci + 4 < nchunk:
                p_load(ci + 4)

        def rms_to_h(l_vec_off, t0, n, hdst, hkeys, D_=D):
            tt = t0 // 512
            xk = [("x", k, tt) for k in range(8)]
            for k in range(8):
                P.op("act", lambda e, k=k: e.activation(out=sq[:, k % 2, 0:n], in_=xT[:, k, t0:t0 + n], func=ACTF.Square),
                     reads=[xk[k]], writes=[("sq", k % 2)])
                P.op("pe", lambda e, k=k: e.matmul(pb[6][:, 0:n], lhsT=ones[:], rhs=sq[:, k % 2, 0:n],
                                                   start=(k == 0), stop=(k == 7)),
                     reads=["ones", ("sq", k % 2)], writes=["pb6"])
            P.op("act", lambda e: e.activation(out=rt[:, 0:n], in_=pb[6][:, 0:n], func=ACTF.Sqrt,
                                               bias=epsb[:], scale=1.0 / D_),
                 reads=["pb6", "epsb"], writes=["rt"])
            P.op("dve", lambda e: e.reciprocal(out=rstd[:, 0:n], in_=rt[:, 0:n]), reads=["rt"], writes=["rstd"])
            for k in range(8):
                P.op("dve", lambda e, k=k: e.scalar_tensor_tensor(
                    out=hdst(k), in0=xT[:, k, t0:t0 + n], scalar=vecs[:, l_vec_off + k:l_vec_off + k + 1],
                    in1=rstd[:, 0:n], op0=ALU.mult, op1=ALU.mult),
                    reads=[xk[k], "rstd", "vecs"], writes=[hkeys(k)])

        epsb = sb("epsb", [128, 1], F32)
        P.op("pool", lambda e: e.memset(epsb[:], EPS), writes=["epsb"])
        negb = sb("negb", [128, 1], F32)
        P.op("pool", lambda e: e.memset(negb[:], -MASK_BIG), writes=["negb"])

        def tap(nm, src_ap_fn, keys):
            if nm in tapout:
                t = P.dma("sp", lambda e: e.dma_start(out=tapout[nm], in_=src_ap_fn()), reads=keys)
                return t

        def ffn(l, f):
            voff = l * NVEC_L + (0 if f == 1 else 16)
            act1 = mts[:, 0, :].rearrange("p (a b) -> p a b", a=2)
            nunits = NFC // 2
            for half in range(2):
                tb0 = half * 1024
                for j in range(2):
                    rms_to_h(voff, tb0 + j * 512, 512,
                             lambda k, j=j: hT[:, k, j * 512:(j + 1) * 512],
                             lambda k, j=j: ("h", k, j))
                gi = [0]
                di = [0]

                def abuf(u):
                    return act if u % 2 == 0 else act1

                def akey(u, jj, j):
                    return ("act", jj, j) if u % 2 == 0 else ("mts", 0)

                def gu_block(u, jj, j, s, wk):
                    gbk = gi[0] % 2
                    gi[0] += 1
                    gb, ub = pb[gbk], pb[2 + gbk]
                    gk, uk = "pb%d" % gbk, "pb%d" % (2 + gbk)
                    ab = abuf(u)
                    for k in range(8):
                        P.op("pe", lambda e, k=k: e.matmul(
                            gb[:, :], lhsT=ring[:, s, k * 128:(k + 1) * 128], rhs=hT[:, k, j * 512:(j + 1) * 512],
                            start=(k == 0), stop=(k == 7)), reads=[wk, ("h", k, j)], writes=[gk])
                    for k in range(8):
                        P.op("pe", lambda e, k=k: e.matmul(
                            ub[:, :], lhsT=ring[:, s, 1024 + k * 128:1024 + (k + 1) * 128],
                            rhs=hT[:, k, j * 512:(j + 1) * 512], start=(k == 0), stop=(k == 7)),
                            reads=[wk, ("h", k, j)], writes=[uk])
                    P.op("act", lambda e: e.activation(out=sg[:, gbk, :], in_=gb[:, :], func=ACTF.Silu),
                         reads=[gk], writes=[("sg", gbk)])
                    P.op("dve", lambda e: e.tensor_tensor(
                        out=ab[:, jj, j * 512:(j + 1) * 512], in0=sg[:, gbk, :], in1=ub[:, :], op=ALU.mult),
                        reads=[("sg", gbk), uk], writes=[akey(u, jj, j)])

                def d_block(u, j, oc, s, wk):
                    dbk = 4 + di[0] % 2
                    di[0] += 1
                    db, dk = pb[dbk], "pb%d" % dbk
                    ab = abuf(u)
                    tt = (tb0 + j * 512) // 512
                    for jj in range(2):
                        P.op("pe", lambda e, jj=jj: e.matmul(
                            db[:, :], lhsT=ring[:, s, jj * 1024 + oc * 128:jj * 1024 + (oc + 1) * 128],
                            rhs=ab[:, jj, j * 512:(j + 1) * 512], start=(jj == 0), stop=(jj == 1)),
                            reads=[wk, akey(u, jj, j)], writes=[dk])
                    P.op("dve", lambda e: e.scalar_tensor_tensor(
                        out=xT[:, oc, tt * 512:(tt + 1) * 512], in0=db[:, :], scalar=0.5,
                        in1=xT[:, oc, tt * 512:(tt + 1) * 512], op0=ALU.mult, op1=ALU.add),
                        reads=[dk, ("x", oc, tt)], writes=[("x", oc, tt)])

                for u in range(nunits + 1):
                    dlist = [(j, oc) for j in range(2) for oc in range(8)] if u >= 1 else []
                    dstate = [None]

                    def emit_d(n, u=u, dlist=dlist, dstate=dstate):
                        for _ in range(n):
                            if not dlist:
                                return
                            if dstate[0] is None:
                                dstate[0] = wload(l, "f%d_d%d" % (f, 2 * (u - 1)))
                            j, oc = dlist.pop(0)
                            d_block(u - 1, j, oc, dstate[0][0], dstate[0][1])

                    if u < nunits:
                        for jj in range(2):
                            s, wk = wload(l, "f%d_gu%d" % (f, 2 * u + jj))
                            for j in range(2):
                                gu_block(u, jj, j, s, wk)
                                emit_d(4)
                    emit_d(16)

        HB = 512

        def evac(eng, out_ap, in_ap, rkeys, wkeys, scale=None):
            if eng == "act":
                if scale is None:
                    return P.op("act", lambda e: e.activation(out=out_ap, in_=in_ap, func=ACTF.Copy), reads=rkeys, writes=wkeys)
                return P.op("act", lambda e: e.activation(out=out_ap, in_=in_ap, func=ACTF.Copy, scale=scale), reads=rkeys, writes=wkeys)
            if scale is None:
                return P.op(eng, lambda e: e.tensor_copy(out=out_ap, in_=in_ap), reads=rkeys, writes=wkeys)
            return P.op(eng, lambda e: e.tensor_scalar(out=out_ap, in0=in_ap, scalar1=scale, scalar2=None, op0=ALU.mult),
                        reads=rkeys, writes=wkeys)

        def proj256(s, wk, coff, bank, half, hb=HB, n=256):
            bkey = "pb%d%s" % (bank, "LR"[half])
            hk = 1 if hb == HB else 0
            for k in range(8):
                P.op("pe", lambda e, k=k: e.matmul(pb[bank][:, half * 256:half * 256 + n],
                                                   lhsT=ring[:, s, coff + k * 128:coff + (k + 1) * 128],
                                                   rhs=hT[:, k, hb:hb + n], start=(k == 0), stop=(k == 7)),
                     reads=[wk, ("h", k, hk)], writes=[bkey])
            return pb[bank][:, half * 256:half * 256 + n], bkey

        def mixer_A(l):
            voff = l * NVEC_L + 8
            s0, wk0 = wload(l, "a0")
            s1, wk1 = wload(l, "a1")
            for tt in range(4):
                t0 = tt * 512
                rms_to_h(voff, t0, 512, lambda k: hT[:, k, 0:512], lambda k: ("h", k, 0))
                for k in range(8):
                    P.op("pe", lambda e, k=k: e.matmul(pb[0][:, :], lhsT=ring[:, s0, k * 128:(k + 1) * 128], rhs=hT[:, k, 0:512],
                                                       start=(k == 0), stop=(k == 7)), reads=[wk0, ("h", k, 0)], writes=["pb0"])
                for k in range(8):
                    P.op("pe", lambda e, k=k: e.matmul(pb[1][:, :], lhsT=ring[:, s0, 1024 + k * 128:1024 + (k + 1) * 128],
                                                       rhs=hT[:, k, 0:512], start=(k == 0), stop=(k == 7)),
                         reads=[wk0, ("h", k, 0)], writes=["pb1"])
                evac("act", kidxT[:, t0:t0 + 512], pb[1][:, :], ["pb1"], [("kidxT", tt)])
                P.op("act", lambda e: e.activation(out=sq[:, 0, :], in_=pb[0][:, :], func=ACTF.Square),
                     reads=["pb0"], writes=[("sq", 0)])
                evac("dve", score[:, 0, 0:512], pb[0][:, :], ["pb0"], [("score", 0)])
                P.op("pe", lambda e: e.matmul(pb[6][:, :], lhsT=ones[:], rhs=sq[:, 0, :], start=True, stop=True),
                     reads=["ones", ("sq", 0)], writes=["pb6"])
                P.op("act", lambda e: e.activation(out=rt[:, :], in_=pb[6][:, :], func=ACTF.Sqrt, bias=epsb[:], scale=1.0 / C),
                     reads=["pb6", "epsb"], writes=["rt"])
                P.op("dve", lambda e: e.reciprocal(out=rstd[:, :], in_=rt[:, :]), reads=["rt"], writes=["rstd"])
                P.op("dve", lambda e, t0=t0: e.scalar_tensor_tensor(
                    out=ckvT[:, t0:t0 + 512], in0=score[:, 0, 0:512], scalar=vecs[:, l * NVEC_L + 24:l * NVEC_L + 25],
                    in1=rstd[:, :], op0=ALU.mult, op1=ALU.mult), reads=[("score", 0), "rstd", "vecs"], writes=[("ckvT", tt)])
                for b in range(4):
                    sbk = tt * 4 + b
                    vb = 2 + b % 2
                    P.op("pe", lambda e, sbk=sbk, vb=vb: e.matmul(pb[vb][:, :], lhsT=ckvT[:, sbk * 128:(sbk + 1) * 128],
                                                                  rhs=ring[:, s1, 0:512], start=True, stop=True),
                         reads=[("ckvT", tt), wk1], writes=["pb%d" % vb])
                    evac("act", vaug[:, sbk, :].rearrange("p (h e) -> p h e", e=65)[:, :, 0:64],
                         pb[vb][:, :].rearrange("p (h d) -> p h d", d=64), ["pb%d" % vb], [("vaug", tt)])

        def hbase(g):
            return HB + 256 * (g % 2)

        def hkey(g):
            return "h%d" % (1 + g % 2)

        def proj256g(g, s, wk, coff, bank):
            hb = hbase(g)
            hk = hkey(g)
            for k in range(8):
                P.op("pe", lambda e, k=k: e.matmul(pb[bank][:, 0:256], lhsT=ring[:, s, coff + k * 128:coff + (k + 1) * 128],
                                                   rhs=hT[:, k, hb:hb + 256], start=(k == 0), stop=(k == 7)),
                     reads=[wk, (hk, k)], writes=["pb%d" % bank])
            return pb[bank][:, 0:256], "pb%d" % bank

        def mix_prep(l, g):
            voff = l * NVEC_L + 8
            t0 = g * 256
            tbs = (2 * g, 2 * g + 1)
            hb = hbase(g)
            hk = hkey(g)
            rms_to_h(voff, t0, 256, lambda k: hT[:, k, hb:hb + 256], lambda k: (hk, k))
            if tbs[1] < 2:
                return
            for i2 in range(2):
                s, wk = wload(l, "bqi%d" % i2)
                for jj in range(2):
                    i = 2 * i2 + jj
                    src, bkey = proj256g(g, s, wk, jj * 1024, i % 2)
                    evac("act", qig[:, i, :], src, [bkey], [("qig", i)])
            sw, wkw = wload(l, "bw")
            for tbl in range(2):
                for k in range(8):
                    P.op("pe", lambda e, k=k, tbl=tbl: e.matmul(pb[0][:, tbl * 8:tbl * 8 + 8],
                                                               lhsT=hT[:, k, hb + tbl * 128:hb + (tbl + 1) * 128],
                                                               rhs=ring[:, sw, k * 8:(k + 1) * 8], start=(k == 0), stop=(k == 7)),
                         reads=[wkw, (hk, k)], writes=["pb0"])
                evac("dve", wtok[:, tbl, :], pb[0][:, tbl * 8:tbl * 8 + 8], ["pb0"], [("wtok", tbl)], scale=IDX_SCALE)
            for tbl in range(2):
                tb = tbs[tbl]
                n_s = 128 * (tb + 1)
                if tb < 2:
                    continue
                for j in range(8):
                    P.op("dve", lambda e, j=j, tbl=tbl: e.tensor_scalar(out=diag[:, j, :], in0=ident[:], scalar1=wtok[:, tbl, j:j + 1],
                                                                        scalar2=None, op0=ALU.mult),
                         reads=["ident", ("wtok", tbl)], writes=[("diag", j)])
                iits = [(sr0, j) for sr0 in range(0, n_s, 512) for j in range(8)]

                def emit_S(it, tbl=tbl, n_s=n_s, iits=iits):
                    sr0, j = iits[it]
                    ncol = min(512, n_s - sr0)
                    i, r = j // 2, j % 2
                    sbank = 2 + it % 2
                    P.op("pe", lambda e: e.matmul(
                        pb[sbank][:, 0:ncol], lhsT=qig[64 * r:64 * r + 64, i, tbl * 128:(tbl + 1) * 128],
                        rhs=kidxT[64 * r:64 * r + 64, sr0:sr0 + ncol], start=True, stop=True),
                        reads=[("qig", i), ("kidxT", sr0 // 512)], writes=["pb%d" % sbank])

                def emit_acc(it, tbl=tbl, n_s=n_s, iits=iits):
                    sr0, j = iits[it]
                    ncol = min(512, n_s - sr0)
                    sbank = 2 + it % 2
                    ri = it % 2
                    P.op("act", lambda e: e.activation(out=rj[:, ri, 0:ncol], in_=pb[sbank][:, 0:ncol], func=ACTF.Relu),
                         reads=["pb%d" % sbank], writes=[("rj", ri)])
                    P.op("pe", lambda e: e.matmul(pb[4][:, 0:ncol], lhsT=diag[:, j, :], rhs=rj[:, ri, 0:ncol],
                                                  start=(j == 0), stop=(j == 7)),
                         reads=[("diag", j), ("rj", ri)], writes=["pb4"])
                    if j == 7:
                        evac("act", score[:, tbl, sr0:sr0 + ncol], pb[4][:, 0:ncol], ["pb4"], [("score", tbl)])

                emit_S(0)
                for it in range(len(iits)):
                    if it + 1 < len(iits):
                        emit_S(it + 1)
                    emit_acc(it)

        def mix_bis(l, g):
            tbs = (2 * g, 2 * g + 1)
            for tbl in range(2):
                tb = tbs[tbl]
                n_s = 128 * (tb + 1)
                if tb < 2:
                    continue
                sk, mk = ("score", tbl), ("mts", tbl)
                sc = score[:, tbl, :]
                mt = mts[:, tbl, :]
                P.op("dve", lambda e, n_s=n_s, sc=sc, mt=mt: e.tensor_scalar(out=mt[:, 0:n_s], in0=sc[:, 0:n_s], scalar1=1.0, scalar2=None,
                                                                             op0=ALU.mult, op1=ALU.max, accum_out=am[:]),
                     reads=[sk], writes=[mk, "am"])
                P.op("dve", lambda e, n_s=n_s, sc=sc, mt=mt: e.tensor_scalar(out=mt[:, 0:n_s], in0=sc[:, 0:n_s], scalar1=-1.0, scalar2=None,
                                                                             op0=ALU.mult, op1=ALU.max, accum_out=aa[:]),
                     reads=[sk], writes=[mk, "aa"])
                P.op("dve", lambda e: e.tensor_tensor(out=am[:], in0=am[:], in1=aa[:], op=ALU.max), reads=["am", "aa"], writes=["am"])
                d0 = tb * 128
                P.op("dve", lambda e, d0=d0, sc=sc: e.tensor_tensor(out=sc[:, d0:d0 + 128], in0=sc[:, d0:d0 + 128],
                                                                    in1=cf[:, C_TRI:C_TRI + 128], op=ALU.mult), reads=[sk, "cf"], writes=[sk])
                P.op("dve", lambda e, d0=d0, sc=sc: e.tensor_tensor(out=sc[:, d0:d0 + 128], in0=sc[:, d0:d0 + 128],
                                                                    in1=cf[:, C_NEG:C_NEG + 128], op=ALU.add), reads=[sk, "cf"], writes=[sk])
                P.op("dve", lambda e: e.tensor_scalar(out=wtab[:], in0=cf[:, C_P2:C_P2 + NIT + 1], scalar1=am[:, 0:1], scalar2=None,
                                                      op0=ALU.mult), reads=["cf", "am"], writes=["wtab"])
                P.op("dve", lambda e: e.memset(probe[:], 0.0), writes=["probe"])
                for it in range(NIT):
                    P.op("dve", lambda e, n_s=n_s, sc=sc, mt=mt: e.tensor_scalar(out=mt[:, 0:n_s], in0=sc[:, 0:n_s], scalar1=probe[:, 0:1],
                                                                                 scalar2=None, op0=ALU.is_ge, op1=ALU.add, accum_out=cnt[:]),
                         reads=[sk, "probe"], writes=[mk, "cnt"])
                    P.op("dve", lambda e: e.tensor_scalar(out=aa[:], in0=cnt[:], scalar1=KSEL - 0.5, scalar2=0.5,
                                                          op0=ALU.is_ge, op1=ALU.subtract), reads=["cnt"], writes=["aa"])
                    P.op("dve", lambda e, it=it: e.scalar_tensor_tensor(out=probe[:], in0=aa[:], scalar=wtab[:, it:it + 1], in1=probe[:],
                                                                        op0=ALU.mult, op1=ALU.add),
                         reads=["aa", "wtab", "probe"], writes=["probe"])
                P.op("dve", lambda e: e.tensor_tensor(out=thr[:], in0=probe[:], in1=wtab[:, NIT:NIT + 1], op=ALU.subtract),
                     reads=["probe", "wtab"], writes=["thr"])
                P.op("dve", lambda e, n_s=n_s, sc=sc, mt=mt: e.tensor_scalar(out=mt[:, 0:n_s], in0=sc[:, 0:n_s], scalar1=thr[:, 0:1],
                                                                             scalar2=None, op0=ALU.is_ge), reads=[sk, "thr"], writes=[mk])

        def mix_tr(l, g):
            tbs = (2 * g, 2 * g + 1)
            for tbl in range(2):
                tb = tbs[tbl]
                if tb >= 2:
                    msrc, mkey = mts[:, tbl, :], ("mts", tbl)
                elif tb == 1:
                    msrc, mkey = cm1, "cm1"
                else:
                    msrc, mkey = trib, "trib"
                for sb0 in range(0, tb + 1, 4):
                    nb = min(4, tb + 1 - sb0)
                    hf = (sb0 // 4) % 2
                    for b in range(nb):
                        P.op("pe", lambda e, b=b, sb0=sb0, hf=hf, msrc=msrc: e.transpose(
                            out=ptp[:, hf * 512 + b * 128:hf * 512 + (b + 1) * 128], in_=msrc[:, (sb0 + b) * 128:(sb0 + b + 1) * 128],
                            identity=ident[:]), reads=[mkey, "ident"], writes=["ptp"])
                    P.op("act", lambda e, sb0=sb0, nb=nb, hf=hf, tbl=tbl: e.activation(
                        out=maskT[:, sb0:sb0 + nb, tbl * 128:(tbl + 1) * 128],
                        in_=ptp[:, hf * 512:hf * 512 + nb * 128].rearrange("p (b t) -> p b t", t=128),
                        func=ACTF.Identity, scale=MASK_BIG, bias=negb[:]),
                        reads=["ptp", "negb"], writes=[("maskT", tbl)])

        def mix_att(l, g):
            tbs = (2 * g, 2 * g + 1)
            for i2 in range(2):
                s, wk = wload(l, "bq%d" % i2)
                for jj in range(2):
                    i = 2 * i2 + jj
                    src, bkey = proj256g(g, s, wk, jj * 1024, i % 2)
                    evac("act", qg[:, i, :], src, [bkey], [("qg", i)])
            sw, wkw = wload(l, "bw")
            for h in range(8):
                i, r = h // 2, h % 2
                bank = h % 2
                P.op("pe", lambda e, i=i, r=r, bank=bank: e.matmul(pb[bank][:, 0:256],
                                                                   lhsT=ring[64 * r:64 * r + 64, sw, 64 + i * 128:64 + (i + 1) * 128],
                                                                   rhs=qg[64 * r:64 * r + 64, i, :], start=True, stop=True),
                     reads=[wkw, ("qg", i)], writes=["pb%d" % bank])
                evac("act", qlat[:, h, :], pb[bank][:, 0:256], ["pb%d" % bank], [("qlat", h)])
            n_sb = tbs[1] + 1
            its = [(sbk, h) for sbk in range(n_sb) for h in range(8)]

            def emit_L(it):
                sbk, h = its[it]
                c0 = 0 if sbk <= tbs[0] else 128
                lb = it % 2
                ttk = sbk // 4
                P.op("pe", lambda e: e.matmul(pb[lb][:, c0:256], lhsT=ckvT[:, sbk * 128:(sbk + 1) * 128],
                                              rhs=qlat[:, h, c0:256], start=True, stop=False),
                     reads=[("ckvT", ttk), ("qlat", h)], writes=["pb%d" % lb])
                P.op("pe", lambda e: e.matmul(pb[lb][:, c0:256], lhsT=ident[:], rhs=maskT[:, sbk, c0:256],
                                              start=False, stop=True),
                     reads=["ident", ("maskT", 0), ("maskT", 1)], writes=["pb%d" % lb])

            def emit_rest(it):
                sbk, h = its[it]
                c0 = 0 if sbk <= tbs[0] else 128
                lb = it % 2
                pi = it % 3
                ttk = sbk // 4
                P.op("act", lambda e: e.activation(out=pt[:, pi, c0:256], in_=pb[lb][:, c0:256], func=ACTF.Exp, scale=ATTN_SCALE),
                     reads=["pb%d" % lb], writes=[("pt", pi)])
                for tbl in range(2):
                    if sbk > tbs[tbl]:
                        continue
                    ab = 2 + 2 * tbl + h // 4
                    P.op("pe", lambda e, tbl=tbl, ab=ab: e.matmul(
                        pb[ab][:, (h % 4) * 65:(h % 4) * 65 + 65], lhsT=pt[:, pi, tbl * 128:(tbl + 1) * 128],
                        rhs=vaug[:, sbk, h * 65:(h + 1) * 65], start=(sbk == 0 and h % 4 == 0), stop=(sbk == tbs[tbl] and h % 4 == 3)),
                        reads=[("pt", pi), ("vaug", ttk)], writes=["pb%d" % ab])

            emit_L(0)
            for it in range(len(its)):
                if it + 1 < len(its):
                    emit_L(it + 1)
                emit_rest(it)

        def mix_rest(l, g):
            t0 = g * 256
            tt = t0 // 512
            for tbl in range(2):
                for hh in range(2):
                    ab = 2 + 2 * tbl + hh
                    evac("act", osb[:, hh * 260:(hh + 1) * 260], pb[ab][:, 0:260], ["pb%d" % ab], ["osb"])
                P.op("dve", lambda e: e.reciprocal(out=rden[:, :], in_=osb[:, :].rearrange("p (h e) -> p h e", e=65)[:, :, 64]),
                     reads=["osb"], writes=["rden"])
                for h in range(8):
                    P.op("dve", lambda e, h=h: e.tensor_scalar(out=atok[:, h * 64:(h + 1) * 64], in0=osb[:, h * 65:h * 65 + 64],
                                                               scalar1=rden[:, h:h + 1], scalar2=None, op0=ALU.mult),
                         reads=["osb", "rden"], writes=["atok"])
                for i in range(4):
                    P.op("pe", lambda e, i=i: e.transpose(out=ptp[:, i * 128:(i + 1) * 128], in_=atok[:, i * 128:(i + 1) * 128], identity=ident[:]),
                         reads=["atok", "ident"], writes=["ptp"])
                evac("act", attnT[:, :, tbl * 128:(tbl + 1) * 128], ptp[:, 0:512].rearrange("p (b t) -> p b t", t=128),
                     ["ptp"], ["attnT"])
            if g == 0:
                P.op("dve", lambda e: e.memset(pbuf[:, :, 0:16], 0.0), writes=[("pbuf", c_) for c_ in range(4)])
            for i2 in range(2):
                s, wk = wload(l, "bp%d" % i2)
                for jj in range(2):
                    gch = 2 * i2 + jj
                    src, bkey = proj256g(g, s, wk, jj * 1024, gch % 2)
                    evac("act", pbuf[:, gch, 16:272], src, [bkey], [("pbuf", gch)])
            spw, wkpw = wload(l, "bpw")
            for gch in range(4):
                w = POOL_W[gch]
                cur = pbuf[:, gch, :]
                pk = ("pbuf", gch)
                P.op("dve", lambda e, cur=cur: e.tensor_tensor(out=ws1[:, 1:272], in0=cur[:, 1:272], in1=cur[:, 0:271], op=ALU.add),
                     reads=[pk], writes=["ws1"])
                fin, fk = ws1, "ws1"
                if w >= 4:
                    P.op("dve", lambda e: e.tensor_tensor(out=ws2[:, 3:272], in0=ws1[:, 3:272], in1=ws1[:, 1:270], op=ALU.add),
                         reads=["ws1"], writes=["ws2"])
                    fin, fk = ws2, "ws2"
                if w >= 8:
                    P.op("dve", lambda e: e.tensor_tensor(out=ws1[:, 7:272], in0=ws2[:, 7:272], in1=ws2[:, 3:268], op=ALU.add),
                         reads=["ws2"], writes=["ws1"])
                    fin, fk = ws1, "ws1"
                if w >= 16:
                    P.op("dve", lambda e: e.tensor_tensor(out=ws2[:, 15:272], in0=ws1[:, 15:272], in1=ws1[:, 7:264], op=ALU.add),
                         reads=["ws1"], writes=["ws2"])
                    fin, fk = ws2, "ws2"
                P.op("dve", lambda e, fin=fin, cur=cur, gch=gch, w=w: e.scalar_tensor_tensor(
                    out=pooledT[:, gch, :], in0=fin[:, 16:272], scalar=1.0 / w, in1=cur[:, 16:272], op0=ALU.mult, op1=ALU.subtract),
                    reads=[fk, pk], writes=[("pooledT", gch)])
                if g == 0:
                    P.op("dve", lambda e, fin=fin, gch=gch: e.tensor_tensor(out=m1[:, 0:16], in0=fin[:, 16:32],
                                                                          in1=cf[:, C_IC + gch * 16:C_IC + (gch + 1) * 16], op=ALU.mult),
                         reads=[fk, "cf"], writes=["m1"])
                    P.op("dve", lambda e, cur=cur, gch=gch: e.tensor_tensor(out=pooledT[:, gch, 0:16], in0=m1[:, 0:16], in1=cur[:, 16:32],
                                                                          op=ALU.subtract), reads=["m1", pk], writes=[("pooledT", gch)])
                P.op("dve", lambda e, cur=cur: e.tensor_copy(out=cur[:, 0:16], in_=cur[:, 256:272]), reads=[pk], writes=[pk])
                bank = 2 + gch % 2
                P.op("pe", lambda e, gch=gch, bank=bank: e.matmul(pb[bank][:, 0:256], lhsT=ring[:, spw, gch * 128:(gch + 1) * 128],
                                                                  rhs=pooledT[:, gch, :], start=True, stop=True),
                     reads=[wkpw, ("pooledT", gch)], writes=["pb%d" % bank])
                evac("dve", mixedT[:, gch, :], pb[bank][:, 0:256], ["pb%d" % bank], [("mixedT", gch)],
                     scale=vecs[:, l * NVEC_L + 25 + gch:l * NVEC_L + 26 + gch])
            for oc in range(8):
                s1_, wk1_ = wload(l, "m1_%d" % oc)
                s2_, wk2_ = wload(l, "m2_%d" % oc)
                b0 = 4 * (oc % 2) if False else 2 * (oc % 2)
                bA, bB, bC, bD = (0, 1, 2, 3) if oc % 2 == 0 else (4, 5, 2, 3)
                ga, gak = proj256g(g, s1_, wk1_, 0, bA)
                gb_, gbk = proj256g(g, s1_, wk1_, 1024, bB)
                for i in range(4):
                    P.op("pe", lambda e, i=i, s2_=s2_, bC=bC: e.matmul(pb[bC][:, 0:256], lhsT=ring[:, s2_, i * 128:(i + 1) * 128],
                                                                    rhs=attnT[:, i, :], start=(i == 0), stop=(i == 3)),
                         reads=[wk2_, "attnT"], writes=["pb%d" % bC])
                for i in range(4):
                    P.op("pe", lambda e, i=i, s2_=s2_, bD=bD: e.matmul(pb[bD][:, 0:256], lhsT=ring[:, s2_, 512 + i * 128:512 + (i + 1) * 128],
                                                                    rhs=mixedT[:, i, :], start=(i == 0), stop=(i == 3)),
                         reads=[wk2_, ("mixedT", i)], writes=["pb%d" % bD])
                P.op("act", lambda e, ga=ga: e.activation(out=sga[:, :], in_=ga, func=ACTF.Sigmoid), reads=[gak], writes=["sga"])
                P.op("act", lambda e, gb_=gb_: e.activation(out=sgb[:, :], in_=gb_, func=ACTF.Sigmoid), reads=[gbk], writes=["sgb"])
                P.op("dve", lambda e, bC=bC: e.tensor_tensor(out=m1[:, :], in0=sga[:, :], in1=pb[bC][:, 0:256], op=ALU.mult),
                     reads=["sga", "pb%d" % bC], writes=["m1"])
                P.op("dve", lambda e, bD=bD: e.tensor_tensor(out=m2[:, :], in0=sgb[:, :], in1=pb[bD][:, 0:256], op=ALU.mult),
                     reads=["sgb", "pb%d" % bD], writes=["m2"])
                P.op("dve", lambda e, oc=oc: e.tensor_tensor(out=mergedT[:, oc, :], in0=m1[:, :], in1=m2[:, :], op=ALU.add),
                     reads=["m1", "m2"], writes=[("mergedT", oc)])
            for oc in range(8):
                s, wk = wload(l, "o_%d" % oc)
                bank = 4 + oc % 2
                for k in range(8):
                    P.op("pe", lambda e, k=k, bank=bank, s=s: e.matmul(pb[bank][:, 0:256], lhsT=ring[:, s, k * 128:(k + 1) * 128],
                                                                  rhs=mergedT[:, k, :], start=(k == 0), stop=(k == 7)),
                         reads=[wk, ("mergedT", k)], writes=["pb%d" % bank])
                P.op("dve", lambda e, oc=oc, bank=bank: e.tensor_tensor(out=xT[:, oc, t0:t0 + 256], in0=pb[bank][:, 0:256],
                                                                        in1=xT[:, oc, t0:t0 + 256], op=ALU.add),
                     reads=["pb%d" % bank, ("x", oc, tt)], writes=[("x", oc, tt)])

        def mixer(l):
            mixer_A(l)
            mix_prep(l, 0)
            mix_bis(l, 0)
            mix_tr(l, 0)
            for g in range(n_groups):
                if g + 1 < n_groups:
                    mix_prep(l, g + 1)
                    mix_bis(l, g + 1)
                mix_att(l, g)
                if g + 1 < n_groups:
                    mix_tr(l, g + 1)
                mix_rest(l, g)

        P.op("pool", lambda e: e.memset(vaug[:, :, :], 1.0), writes=[("vaug", c_) for c_ in range(4)])

        final_toks = []
        for sq_i in range(n_seq):
            for k in range(8):
                P.dma("sp", lambda e, k=k, sq_i=sq_i: e.dma_start(out=xT[:, k, :], in_=xin[sq_i, k * 128:(k + 1) * 128, :]),
                      writes=[("x", k, tt) for tt in range(4)])
            for l in range(depth):
                ffn(l, 1)
                if stop_after == "ffn1":
                    break
                mixer(l)
                if stop_after == "mix":
                    break
                ffn(l, 2)
            voff = depth * NVEC_L
            for tt in range(4):
                rms_to_h_dummy = None
                xk = [("x", k, tt) for k in range(8)]
                t0 = tt * 512
                for k in range(8):
                    P.op("act", lambda e, k=k, t0=t0: e.activation(out=sq[:, k % 2, :], in_=xT[:, k, t0:t0 + 512], func=ACTF.Square),
                         reads=[xk[k]], writes=[("sq", k % 2)])
                    P.op("pe", lambda e, k=k: e.matmul(pb[6][:, :], lhsT=ones[:], rhs=sq[:, k % 2, :],
                                                       start=(k == 0), stop=(k == 7)),
                         reads=["ones", ("sq", k % 2)], writes=["pb6"])
                P.op("act", lambda e: e.activation(out=rt[:, :], in_=pb[6][:, :], func=ACTF.Sqrt, bias=epsb[:], scale=1.0 / D),
                     reads=["pb6", "epsb"], writes=["rt"])
                P.op("dve", lambda e: e.reciprocal(out=rstd[:, :], in_=rt[:, :]), reads=["rt"], writes=["rstd"])
                for k in range(8):
                    oi = k % 2
                    if stop_after is None:
                        P.op("dve", lambda e, k=k, t0=t0, oi=oi: e.scalar_tensor_tensor(
                            out=score[:, 1, oi * 512:(oi + 1) * 512], in0=xT[:, k, t0:t0 + 512], scalar=vecs[:, voff + k:voff + k + 1],
                            in1=rstd[:, :], op0=ALU.mult, op1=ALU.mult),
                            reads=[xk[k], "rstd", "vecs"], writes=[("ostg", oi), ("score", 1)])
                    else:
                        P.op("dve", lambda e, k=k, t0=t0, oi=oi: e.tensor_copy(out=score[:, 1, oi * 512:(oi + 1) * 512], in_=xT[:, k, t0:t0 + 512]),
                             reads=[xk[k]], writes=[("ostg", oi)])
                    final_toks.append(P.dma("sp", lambda e, k=k, t0=t0, oi=oi, sq_i=sq_i: e.dma_start(
                        out=yout[sq_i, k * 128:(k + 1) * 128, t0:t0 + 512], in_=score[:, 1, oi * 512:(oi + 1) * 512]),
                        reads=[("ostg", oi)]))
        P.wait_all("sp", final_toks)
        P.emit()
    return nc, P


N_CORES = 8


def pack_weights(inp, depth):
    off, wtot = tile_offsets(depth)
    wall = np.zeros((128, wtot), np.float32)
    for l in range(depth):
        for name, arr in layer_tiles(inp, l):
            o = off[(l, name)]
            assert arr.shape == (128, tile_widths()[name]), (name, arr.shape)
            wall[:, o:o + arr.shape[1]] = arr
    return wall


def kernel(**inputs):
    inp = {k: np.asarray(v) for k, v in inputs.items()}
    x = inp["x"]
    B = x.shape[0]
    n_seq = B // N_CORES
    nc, _ = build(n_seq, DEPTH)
    wall = pack_weights(inp, DEPTH)
    consts = make_consts()
    vecs = make_vecs(inp, DEPTH)
    xT = np.ascontiguousarray(x.transpose(0, 2, 1))
    in_maps = []
    for c in range(N_CORES):
        in_maps.append({"xT": xT[c * n_seq:(c + 1) * n_seq], "wall": wall, "consts": consts, "vecs": vecs})
    res = run_bass_kernel_spmd(nc, in_maps, core_ids=list(range(N_CORES)))
    yT = np.concatenate([r["yT"] for r in res.results], axis=0)
    return np.ascontiguousarray(yT.transpose(0, 2, 1))
```
